# Optimizing a Trainium2 kernel written in Bass

```python
import jax, jax.numpy as jnp
from jax import lax
import numpy as np

D_MODEL = 1024
BATCH = 8
SEQ = 4096
DEPTH = 2

N_A_LAYERS = DEPTH // 2
N_B_LAYERS = DEPTH - N_A_LAYERS
N_DENSE_LAYERS = (DEPTH + 1) // 2
N_MOE_LAYERS = DEPTH // 2

PLE_DIM = 256
POOL_WINDOWS = (2, 4, 8, 16)
N_POOL_GROUPS = len(POOL_WINDOWS)
POOL_GROUP = D_MODEL // N_POOL_GROUPS

HEAD_DIM = 64
N_HEADS = D_MODEL // HEAD_DIM
N_KV_HEADS = 4
Q_PER_KV = N_HEADS // N_KV_HEADS
WINDOW = 128
BLOCK = 128
ROPE_THETA = 10000.0

D_FF = ((8 * D_MODEL // 3 + 255) // 256) * 256
N_EXPERTS = 8
TOP_K = 2
D_FF_EXPERT = D_MODEL
EPS = 1e-6

kernel_name = "yoco_pool_swa_sink_moe_hybrid"


def rmsnorm(x, g):
    xf = x.astype(jnp.float32)
    y = xf * lax.rsqrt(jnp.mean(xf * xf, axis=-1, keepdims=True) + EPS) * g.astype(jnp.float32)
    return y.astype(x.dtype)


def rope_tables(seq):
    inv = ROPE_THETA ** (-jnp.arange(0, HEAD_DIM, 2, dtype=jnp.float32) / HEAD_DIM)
    ang = jnp.arange(seq, dtype=jnp.float32)[:, None] * inv[None, :]
    return jnp.cos(ang), jnp.sin(ang)


def apply_rope(x, cos, sin):
    xf = x.astype(jnp.float32)
    x1, x2 = jnp.split(xf, 2, axis=-1)
    c = cos[None, :, None, :]
    s = sin[None, :, None, :]
    return jnp.concatenate([x1 * c - x2 * s, x2 * c + x1 * s], axis=-1).astype(x.dtype)


def pool_mixer(u, w, scale):
    seq = u.shape[1]
    uf = u.astype(jnp.float32)
    cs = jnp.cumsum(uf, axis=1)
    pos = jnp.arange(1, seq + 1, dtype=jnp.float32)
    outs = []
    for g, win in enumerate(POOL_WINDOWS):
        c0 = g * POOL_GROUP
        ug = uf[..., c0:c0 + POOL_GROUP]
        csg = cs[..., c0:c0 + POOL_GROUP]
        lag = jnp.pad(csg[:, :-win], ((0, 0), (win, 0), (0, 0)))
        cnt = jnp.minimum(pos, float(win))[None, :, None]
        d = ((csg - lag) / cnt - ug).astype(u.dtype)
        outs.append(jnp.einsum("bsc,cd->bsd", d, w[g]))
    return jnp.concatenate(outs, axis=-1) * scale


def shared_kv(h, kv_norm, w_kv, k_norm, cos, sin):
    b, s, _ = h.shape
    kv = rmsnorm(h, kv_norm) @ w_kv
    k, v = jnp.split(kv, 2, axis=-1)
    k = k.reshape(b, s, N_KV_HEADS, HEAD_DIM)
    v = v.reshape(b, s, N_KV_HEADS, HEAD_DIM)
    k = apply_rope(rmsnorm(k, k_norm), cos, sin)
    return k, v


def sliding_sink_attention(q, k, v, sinks):
    b, s, _, _ = q.shape
    nb = s // BLOCK
    qb = q.reshape(b, nb, BLOCK, N_KV_HEADS, Q_PER_KV, HEAD_DIM).transpose(1, 0, 2, 3, 4, 5)

    def band(t):
        tb = t.reshape(b, nb, BLOCK, N_KV_HEADS, HEAD_DIM).transpose(1, 0, 2, 3, 4)
        prev = jnp.concatenate([jnp.zeros_like(tb[:1]), tb[:-1]], axis=0)
        return jnp.concatenate([prev, tb], axis=2)

    kb, vb = band(k), band(v)
    qi = jnp.arange(BLOCK)[:, None]
    kj = jnp.arange(2 * BLOCK)[None, :]
    diff = BLOCK + qi - kj
    in_window = (diff >= 0) & (diff < WINDOW)
    sink = sinks.astype(jnp.float32).reshape(N_KV_HEADS, Q_PER_KV)[None, :, :, None, None]
    scale = HEAD_DIM ** -0.5

    def block_fn(args):
        qn, kn, vn, n = args
        sc = jnp.einsum("bqhgd,bkhd->bhgqk", qn.astype(jnp.float32), kn.astype(jnp.float32)) * scale
        key_ok = (n * BLOCK - BLOCK + kj) >= 0
        mask = in_window & key_ok
        sc = jnp.where(mask[None, None, None], sc, -jnp.inf)
        m = jnp.maximum(jnp.max(sc, axis=-1, keepdims=True), sink)
        pr = jnp.exp(sc - m)
        denom = jnp.sum(pr, axis=-1, keepdims=True) + jnp.exp(sink - m)
        out = jnp.einsum("bhgqk,bkhd->bqhgd", pr / denom, vn.astype(jnp.float32))
        return out.astype(qn.dtype)

    o = lax.map(block_fn, (qb, kb, vb, jnp.arange(nb)))
    return o.transpose(1, 0, 2, 3, 4, 5).reshape(b, s, N_HEADS * HEAD_DIM)


def swiglu(u, w_gu, w_down):
    a, g = jnp.split(u @ w_gu, 2, axis=-1)
    return (jax.nn.silu(a) * g) @ w_down


def moe_swiglu(u, router_w, router_b, we_gu, we_down):
    b, s, d = u.shape
    t = u.reshape(b * s, d)
    logits = (t @ router_w).astype(jnp.float32) + router_b.astype(jnp.float32)
    top_v, top_i = lax.top_k(logits, TOP_K)
    top_w = jax.nn.softmax(top_v, axis=-1)
    gates = jnp.sum(jax.nn.one_hot(top_i, N_EXPERTS, dtype=jnp.float32) * top_w[..., None], axis=1)
    gates = gates.astype(u.dtype)
    y = jnp.zeros_like(t)
    for e in range(N_EXPERTS):
        y = y + gates[:, e:e + 1] * swiglu(t, we_gu[e], we_down[e])
    return y.reshape(b, s, d)


def setup_inputs(seed: int = 0) -> dict:
    key = jax.random.key(seed)
    ks = iter(jax.random.split(key, 32))

    def nrm(shape, scale):
        return jax.random.normal(next(ks), shape, jnp.float32) * scale

    def gain(shape):
        return 1.0 + nrm(shape, 0.02)

    D = D_MODEL
    return {
        "x": nrm((BATCH, SEQ, D), 1.0),
        "p": nrm((DEPTH, BATCH, SEQ, PLE_DIM), 1.0),
        "pool_norm": gain((N_A_LAYERS, D)),
        "pool_w": nrm((N_A_LAYERS, N_POOL_GROUPS, POOL_GROUP, POOL_GROUP), POOL_GROUP ** -0.5),
        "pool_scale": 1.0 + nrm((N_A_LAYERS, D), 0.1),
        "kv_norm": gain((D,)),
        "w_kv": nrm((D, 2 * N_KV_HEADS * HEAD_DIM), D ** -0.5),
        "k_norm": gain((HEAD_DIM,)),
        "attn_norm": gain((N_B_LAYERS, D)),
        "w_q": nrm((N_B_LAYERS, D, N_HEADS * HEAD_DIM), D ** -0.5),
        "q_norm": gain((N_B_LAYERS, HEAD_DIM)),
        "sinks": nrm((N_B_LAYERS, N_HEADS), 1.0),
        "w_o": nrm((N_B_LAYERS, N_HEADS * HEAD_DIM, D), (N_HEADS * HEAD_DIM) ** -0.5),
        "ffn_norm": gain((DEPTH, D)),
        "w_gu": nrm((N_DENSE_LAYERS, D, 2 * D_FF), D ** -0.5),
        "w_down": nrm((N_DENSE_LAYERS, D_FF, D), D_FF ** -0.5),
        "router_w": nrm((N_MOE_LAYERS, D, N_EXPERTS), D ** -0.5),
        "router_b": nrm((N_MOE_LAYERS, N_EXPERTS), 0.01),
        "we_gu": nrm((N_MOE_LAYERS, N_EXPERTS, D, 2 * D_FF_EXPERT), D ** -0.5),
        "we_down": nrm((N_MOE_LAYERS, N_EXPERTS, D_FF_EXPERT, D), D_FF_EXPERT ** -0.5),
        "ple_gate_norm": gain((DEPTH, D)),
        "ple_gate_w": nrm((DEPTH, D, D), D ** -0.5),
        "ple_w": nrm((DEPTH, PLE_DIM, D), PLE_DIM ** -0.5),
    }


def reference(x, p, pool_norm, pool_w, pool_scale, kv_norm, w_kv, k_norm, attn_norm, w_q,
              q_norm, sinks, w_o, ffn_norm, w_gu, w_down, router_w, router_b, we_gu, we_down,
              ple_gate_norm, ple_gate_w, ple_w):
    b, s, d = x.shape
    cos, sin = rope_tables(s)
    h = x
    k_sh = v_sh = None
    for i in range(DEPTH):
        if i < N_A_LAYERS:
            h = h + pool_mixer(rmsnorm(h, pool_norm[i]), pool_w[i], pool_scale[i])
        else:
            j = i - N_A_LAYERS
            if j == 0:
                k_sh, v_sh = shared_kv(h, kv_norm, w_kv, k_norm, cos, sin)
            q = (rmsnorm(h, attn_norm[j]) @ w_q[j]).reshape(b, s, N_HEADS, HEAD_DIM)
            q = apply_rope(rmsnorm(q, q_norm[j]), cos, sin)
            h = h + sliding_sink_attention(q, k_sh, v_sh, sinks[j]) @ w_o[j]
        u = rmsnorm(h, ffn_norm[i])
        if i % 2 == 0:
            h = h + swiglu(u, w_gu[i // 2], w_down[i // 2])
        else:
            h = h + moe_swiglu(u, router_w[i // 2], router_b[i // 2], we_gu[i // 2], we_down[i // 2])
        gate = jax.nn.sigmoid(rmsnorm(h, ple_gate_norm[i]) @ ple_gate_w[i])
        h = h + gate * (p[i].astype(h.dtype) @ ple_w[i])
    return h
```

```python
from contextlib import ExitStack

import ml_dtypes
import numpy as np

import concourse.bass as bass
import concourse.mybir as mybir
from concourse.bass_utils import run_bass_kernel_spmd

F32 = mybir.dt.float32
BF16 = mybir.dt.bfloat16
AF = mybir.ActivationFunctionType
ALU = mybir.AluOpType
AX = mybir.AxisListType

D = 1024
SEQ = 4096
TM = 512
NSUB = 4
DFF = 2816
NJ = DFF // 128
NE = 8
EPS = 1e-6
DEBUG = False
SIMLOG = []
REMAP = {}
SLOT = 4096
BIAS = 0.3

PERM = []
for _c in range(8):
    for _half in range(2):
        if _c < 4:
            PERM.append(_c + 4 * _half)
        else:
            PERM.append(8 + (_c - 4) + 4 * _half)


class Buf:
    __slots__ = ("name", "w", "r", "wsem", "wcnt", "rsem", "rcnt", "multi")

    def __init__(self, name, multi=False):
        self.name = name
        self.w = {}
        self.r = {}
        self.wsem = None
        self.wcnt = 0
        self.rsem = None
        self.rcnt = 0
        self.multi = multi


class Tracker:
    def __init__(self, nc, stack, same_engine_sync=True):
        self.nc = nc
        self.stack = stack
        self.same = same_engine_sync
        self.engs = {"pe": nc.tensor, "act": nc.scalar, "dve": nc.vector, "pool": nc.gpsimd, "sp": nc.sync}
        self.sem = {}
        self.seq = {}
        self.waited = {k: {} for k in self.engs}
        self.nsem = 0
        for k in ("pe", "act", "dve", "pool"):
            self.sem[k] = self.new_sem("e_" + k)
            self.seq[k] = 0
        self.ninst = 0
        self.nwait = 0
        self.remap = dict(REMAP)
        self.clock = {k: 0.0 for k in self.engs}
        self.tt = {}
        self.dma_free = 0.0
        self.busy = {}
        self.gaps = {}
        self.act_tbl = None
        self.tag = ''

    def new_sem(self, name):
        self.nsem += 1
        return self.stack.enter_context(self.nc.semaphore("%s_%d" % (name, self.nsem)))

    def _wait(self, e, tok):
        sem, val = tok
        own = self.sem.get(e)
        if own is not None and sem is own and not self.same:
            return
        key = id(sem)
        if self.waited[e].get(key, 0) >= val:
            return
        self.engs[e].wait_ge(sem, val)
        self.waited[e][key] = val
        self.nwait += 1

    def _deps(self, e, reads, writes):
        for b in reads:
            for tok in b.w.values():
                self._wait(e, tok)
        for b in writes:
            if not b.multi:
                for tok in b.w.values():
                    self._wait(e, tok)
            for tok in b.r.values():
                self._wait(e, tok)

    def _finish(self, reads, writes, tok):
        key = id(tok[0])
        for b in reads:
            old = b.r.get(key)
            if old is None or old[1] < tok[1]:
                b.r[key] = tok
        for b in writes:
            if b.multi:
                old = b.w.get(key)
                if old is None or old[1] < tok[1]:
                    b.w[key] = tok
            else:
                b.w = {key: tok}
                b.r = {}
        self.ninst += 1

    COST = {"pe": 0.3, "act": 0.8, "dve": 0.75, "pool": 1.2}

    def ready_time(self, reads, writes):
        t = 0.0
        for b in reads:
            for tok in b.w.values():
                t = max(t, self.tt.get((id(tok[0]), tok[1]), 0.0))
        for b in writes:
            for tok in list(b.w.values()) + list(b.r.values()):
                t = max(t, self.tt.get((id(tok[0]), tok[1]), 0.0))
        return t

    def op(self, e, fn, reads=(), writes=(), cost=None, tbl=None):
        e = self.remap.get(e, e)
        if tbl is not None and tbl != self.act_tbl:
            cost = (self.COST[e] if cost is None else cost) + 1.28
            self.act_tbl = tbl
        rt = self.ready_time(reads, writes)
        self._deps(e, reads, writes)
        ins = fn(self.engs[e])
        self.seq[e] += 1
        ins.then_inc(self.sem[e], 1)
        tok = (self.sem[e], self.seq[e])
        start = max(self.clock[e], rt + 0.1)
        if e == "pe" and start > self.clock[e]:
            self.gaps[self.tag] = self.gaps.get(self.tag, 0.0) + (start - self.clock[e])
        end = start + (self.COST[e] if cost is None else cost)
        self.busy[e] = self.busy.get(e, 0.0) + (end - start)
        self.clock[e] = end
        self.tt[(id(tok[0]), tok[1])] = end + (0.2 if e == "pe" else 0.05)
        self._finish(reads, writes, tok)
        return tok

    def dma(self, q, outs_ins, reads=(), writes=(), sem_on=None, **kw):
        rt = self.ready_time(reads, writes)
        self._deps(q, reads, writes)
        b, kind = sem_on
        tok = None
        issue = max(self.clock[q], rt + 0.1)
        nbytes = 0
        for out_ap, in_ap in outs_ins:
            nbytes += out_ap.nbytes() if callable(getattr(out_ap, "nbytes", None)) else 0
            issue += 0.6
            ins = self.engs[q].dma_start(out=out_ap, in_=in_ap, **kw)
            if kind == "w":
                if b.wsem is None:
                    b.wsem = self.new_sem("w_" + b.name)
                b.wcnt += 16
                ins.then_inc(b.wsem, 16)
                tok = (b.wsem, b.wcnt)
            else:
                if b.rsem is None:
                    b.rsem = self.new_sem("r_" + b.name)
                b.rcnt += 16
                ins.then_inc(b.rsem, 16)
                tok = (b.rsem, b.rcnt)
        self.clock[q] = issue
        done = max(issue + 2.0, self.dma_free) + nbytes / 3.0e5
        self.dma_free = done
        self.tt[(id(tok[0]), tok[1])] = done
        self._finish(reads, writes, tok)
        return tok

    def absorb(self, dst, srcs):
        for b in srcs:
            for tok in list(b.w.values()) + list(b.r.values()):
                key = id(tok[0])
                old = dst.r.get(key)
                if old is None or old[1] < tok[1]:
                    dst.r[key] = tok

    def wait_all(self, e, bufs):
        for b in bufs:
            for tok in b.w.values():
                self._wait(e, tok)
            for tok in b.r.values():
                self._wait(e, tok)


def build(n_macro=SEQ // TM, stop_after="ple1", same_engine_sync=True):
    STAGES = ["load", "pool", "ffn", "ple0", "attn", "moe", "ple1"]
    last_stage = STAGES.index(stop_after)
    seq = n_macro * TM
    nc = bass.Bass("TRN2", target_bir_lowering=False)

    def din(name, shape, dt=F32):
        return nc.dram_tensor(name, list(shape), dt, kind="ExternalInput").ap()

    x_d = din("x", [seq, D])
    p_d = [din("p0", [seq, 256]), din("p1", [seq, 256])]
    cos_d = din("cos", [seq, 32])
    sin_d = din("sin", [seq, 32])
    ident_d = din("ident", [128, 128], BF16)
    poolA_d = din("poolA", [12, 128, 128], BF16)
    masks_d = din("masks", [128, 256], BF16)
    pool_norm_d = din("pool_norm", [D])
    pool_w_d = din("pool_w", [4, 256, 256])
    pool_scale_d = din("pool_scale", [D])
    kv_norm_d = din("kv_norm", [D])
    w_kv_d = din("w_kv", [D, 512])
    k_norm_d = din("k_norm", [64])
    attn_norm_d = din("attn_norm", [D])
    w_q_d = din("w_q", [D, D])
    q_norm_d = din("q_norm", [64])
    sinks_d = din("sinks", [16])
    w_o_d = din("w_o", [D, D])
    ffn_norm_d = din("ffn_norm", [2, D])
    w_gu_d = din("w_gu", [D, 2 * DFF])
    w_down_d = din("w_down", [DFF, D])
    router_w_d = din("router_w", [D, NE])
    router_b_d = din("router_b", [NE])
    we_gu_d = din("we_gu", [NE, D, 2 * D])
    we_down_d = din("we_down", [NE, D, D])
    pgn_d = din("ple_gate_norm", [2, D])
    pgw_d = din("ple_gate_w", [2, D, D])
    plew_d = din("ple_w", [2, 256, D])
    out_d = nc.dram_tensor("out", [seq, D], F32, kind="ExternalOutput").ap()

    def dscr(name, shape):
        return nc.dram_tensor(name, list(shape), BF16, kind="Internal").ap()

    s_gu = dscr("s_gu", [128, 8, NJ * 256]); B_s_gu = Buf("s_gu", multi=True)
    s_down = dscr("s_down", [128, NJ, D]); B_s_down = Buf("s_down", multi=True)
    s_pg = [dscr("s_pg%d" % l, [128, 8, D]) for l in range(2)]; B_s_pg = [Buf("s_pg%d" % l, multi=True) for l in range(2)]
    s_kv = dscr("s_kv", [128, 8, 512]); B_s_kv = Buf("s_kv", multi=True)
    s_q = dscr("s_q", [128, 8, D]); B_s_q = Buf("s_q", multi=True)
    s_o = dscr("s_o", [128, 8, D]); B_s_o = Buf("s_o", multi=True)
    s_egu = [dscr("s_egu%d" % e, [128, 8, 2 * D]) for e in range(NE)]; B_s_egu = [Buf("s_egu%d" % e, multi=True) for e in range(NE)]
    s_ed = [dscr("s_ed%d" % e, [128, 8, D]) for e in range(NE)]; B_s_ed = [Buf("s_ed%d" % e, multi=True) for e in range(NE)]

    s_plew = [dscr("s_plew%d" % l, [128, 2, D]) for l in range(2)]; B_s_plew = [Buf("s_plew%d" % l, multi=True) for l in range(2)]

    with ExitStack() as st:
        T = Tracker(nc, st, same_engine_sync)

        def sb(name, shape, dt):
            return st.enter_context(nc.sbuf_tensor("sb_" + name, list(shape), dt))

        NRA, NRB = 5, 3
        ringA = [sb("ringA%d" % i, [128, SLOT], BF16) for i in range(NRA)]; BringA = [Buf("ringA%d" % i) for i in range(NRA)]
        ringB = [sb("ringB%d" % i, [128, SLOT], BF16) for i in range(NRB)]; BringB = [Buf("ringB%d" % i) for i in range(NRB)]
        hT = [sb("h%d" % c, [128, NSUB, D], F32) for c in range(2)]
        Bh = [[Buf("h%d_%d" % (c, s)) for s in range(NSUB)] for c in range(2)]
        u = [sb("u%d" % i, [128, D], BF16) for i in range(7)]; Bu = [Buf("u%d" % i) for i in range(7)]
        uTa = sb("uTa", [128, 8, TM], BF16); BuTa = [Buf("uTa%d" % i) for i in range(NSUB)]
        uTb = sb("uTb", [128, 8, TM], BF16); BuTb = [Buf("uTb%d" % i) for i in range(NSUB)]
        QT = sb("QT", [128, 8, TM], BF16); BQT = Buf("QT")
        hm = [sb("hm%d" % i, [128, 8, TM], BF16) for i in range(2)]; Bhm = [Buf("hm%d" % i) for i in range(2)]
        dT = sb("dT", [128, 8, 128], BF16); BdT = Buf("dT")
        junk = sb("junk", [128, D], BF16)
        junk2 = sb("junk2", [128, D], BF16)
        th = [sb("th%d" % i, [128, TM], F32) for i in range(2)]; Bth = [Buf("th%d" % i) for i in range(2)]
        mm = [sb("mm%d" % i, [128, TM], F32) for i in range(2)]; Bmm = [Buf("mm%d" % i) for i in range(2)]
        pb = sb("pb", [128, NSUB * 256], BF16); Bpb = Buf("pb")
        pT = sb("pT", [128, 2, TM], BF16); BpT_ = Buf("pT")
        qf = sb("qf", [128, D], F32); Bqf = Buf("qf")
        rtt = sb("rtt", [128, D], F32); Brt = [Buf("rt0"), Buf("rt1")]
        kf = sb("kf", [128, 256], F32); Bkf = Buf("kf")
        qr = sb("qr", [128, D], BF16); Bqr = Buf("qr")
        kr = sb("kr", [128, 256], BF16); Bkr = Buf("kr")
        KT = sb("KT", [128, 2, 5 * 128], BF16); BK = [Buf("K%d" % i) for i in range(5)]
        Vaug = sb("Vaug", [128, 5, 4, 66], BF16); BV = [Buf("V%d" % i) for i in range(5)]
        PT = [sb("PT%d" % i, [128, 1024], BF16) for i in range(2)]; BPT = [Buf("PT%d" % i) for i in range(2)]
        Ot = sb("Ot", [128, D], BF16); BO = Buf("Ot")
        OT = sb("OT", [128, 8, 128], BF16); BOT = Buf("OT")
        ident = sb("ident", [128, 128], BF16); Bident = Buf("ident")
        poolA = sb("poolA", [128, 12, 128], BF16); BpoolA = Buf("poolA")
        masks = sb("masks", [128, 2, 128], BF16); Bmasks = Buf("masks")
        wpool = sb("wpool", [128, 4, 2, 256], BF16); Bwpool = Buf("wpool")
        wr = sb("wr", [128, 8, NE], BF16); Bwr = Buf("wr")
        wrf = sb("wrf", [128, 8, NE], F32); Bwrf = Buf("wrf")
        gcol = sb("gcol", [128, 7, 8], F32); Bgcol = Buf("gcol")
        gq_bc = sb("gq_bc", [128, 64], F32); gk_bc = sb("gk_bc", [128, 64], F32); Bgqk = Buf("gqk")
        rb_bc = sb("rb_bc", [128, NE], F32); Brb = Buf("rb")
        sinks_bc = sb("sinks_bc", [128, 16], F32); Bsinks = Buf("sinks")
        esink = sb("esink", [128, 16], F32); Besink = Buf("esink")
        negc = sb("negc", [128, 1], F32); Bnegc = Buf("negc")
        mqk = sb("mqk", [128, 2], F32); Bmqk = Buf("mqk")
        cs = sb("cs", [128, NSUB, 32], F32); sn = sb("sn", [128, NSUB, 32], F32); Bcs = Buf("cs"); Bsn = Buf("sn")
        rtab = sb("rtab", [128, 8, NSUB, 32], F32); Brtab = Buf("rtab")
        ssA = sb("ssA", [128, NSUB], F32); BssA = Buf("ssA"); rstdA = sb("rstdA", [128, NSUB], F32); BrstdA = Buf("rstdA")
        ssB = sb("ssB", [128, NSUB], F32); BssB = Buf("ssB"); rstdB = sb("rstdB", [128, NSUB], F32); BrstdB = Buf("rstdB")
        ssN = {k_: sb("ssN" + k_, [128, NSUB], F32) for k_ in "AB"}; rstdN = {k_: sb("rstdN" + k_, [128, NSUB], F32) for k_ in "AB"}
        BssN = {k_: [Buf("ssN%s%d" % (k_, i)) for i in range(NSUB)] for k_ in "AB"}
        BrstdN = {k_: [Buf("rstdN%s%d" % (k_, i)) for i in range(NSUB)] for k_ in "AB"}
        ssq = sb("ssq", [128, 16], F32); Bssq = Buf("ssq")
        rq = sb("rq", [128, 16], F32); Brq = Buf("rq")
        den = sb("den", [128, 4], F32); Bden = Buf("den")
        rden = sb("rden", [128, 4], F32); Brden = Buf("rden")
        Lg = sb("Lg", [128, NSUB, NE], F32); BLg = Buf("Lg")
        m8 = sb("m8", [128, NSUB, 8], F32); Bm8 = Buf("m8")
        nv1 = sb("nv1", [128, NSUB], F32); Bnv1 = Buf("nv1")
        ex = sb("ex", [128, NSUB, NE], F32); Bex = Buf("ex")
        msk = sb("msk", [128, NSUB, NE], F32); Bmsk = Buf("msk")
        gden = sb("gden", [128, NSUB], F32); Bgden = Buf("gden")
        gates = sb("gates", [128, NSUB, NE], F32); Bgates = Buf("gates")

        psT = st.enter_context(nc.psum_tensor("psT", [128, 2, 1024], BF16)); BpT = [Buf("psT%d" % i) for i in range(2)]
        psF = st.enter_context(nc.psum_tensor("psF", [128, 6, 512], F32)); BpF = [Buf("psF%d" % i) for i in range(6)]
        pst = {"a1": 0, "a2": 0, "b1": 0, "ringA": 0, "cvt": 0, "th": 0, "pt": 0}

        def psA():
            i = pst["a1"]; pst["a1"] = (i + 1) % 4
            return i

        def psA_pair():
            i = pst["a2"]; pst["a2"] = (i + 1) % 2
            return 2 * i

        def psB():
            i = pst["b1"]; pst["b1"] = (i + 1) % 2
            return 4 + i

        T.dma("sp", [(ident[:], ident_d[:, :])], writes=[Bident], sem_on=(Bident, "w"))
        T.dma("sp", [(poolA[:], poolA_d.rearrange("a p t -> p a t"))], writes=[BpoolA], sem_on=(BpoolA, "w"))
        T.dma("sp", [(masks[:].rearrange("p a b -> p (a b)"), masks_d[:, :])], writes=[Bmasks], sem_on=(Bmasks, "w"))
        gsrc = [pool_norm_d, ffn_norm_d[0, :], ffn_norm_d[1, :], pgn_d[0, :], pgn_d[1, :], kv_norm_d, attn_norm_d]
        G_POOL, G_FFN0, G_FFN1, G_PG0, G_PG1, G_KV, G_Q = range(7)
        with nc.allow_non_contiguous_dma(reason="one-time 4KiB gain-vector transposes"):
            T.dma("sp", [(gcol[:, i, :], g.rearrange("(kc p) -> p kc", p=128)) for i, g in enumerate(gsrc)],
                  writes=[Bgcol], sem_on=(Bgcol, "w"))
        T.dma("sp", [(gq_bc[:], q_norm_d.partition_broadcast(128)), (gk_bc[:], k_norm_d.partition_broadcast(128))],
              writes=[Bgqk], sem_on=(Bgqk, "w"))
        T.dma("sp", [(rb_bc[:], router_b_d.partition_broadcast(128))], writes=[Brb], sem_on=(Brb, "w"))
        T.dma("sp", [(sinks_bc[:], sinks_d.partition_broadcast(128))], writes=[Bsinks], sem_on=(Bsinks, "w"))
        T.op("dve", lambda e: e.tensor_reduce(out=mqk[:, 0:1], in_=gq_bc[:], axis=AX.X, op=ALU.max, apply_absolute_value=True),
             reads=[Bgqk], writes=[Bmqk])
        T.op("dve", lambda e: e.tensor_reduce(out=mqk[:, 1:2], in_=gk_bc[:], axis=AX.X, op=ALU.max, apply_absolute_value=True),
             reads=[Bgqk], writes=[Bmqk])
        T.op("dve", lambda e: e.tensor_scalar(out=negc[:], in0=mqk[:, 0:1], scalar1=-8.0, scalar2=mqk[:, 1:2], op0=ALU.mult, op1=ALU.mult),
             reads=[Bmqk], writes=[Bnegc])
        T.op("act", lambda e: e.activation(out=esink[:], in_=sinks_bc[:], func=AF.Exp, bias=negc[:, 0:1]),
             reads=[Bsinks, Bnegc], writes=[Besink])
        T.op("pool", lambda e: e.memset(Vaug[:].rearrange("p a b c -> p (a b c)"), 1.0), writes=BV)

        allr = ringA + ringB
        Ballr = BringA + BringB
        in_slots = [0, 1, 2, 3]
        out_slots = [4, 5, 6, 7]
        pre = {"i": 0}
        cvt_engs = ["act", "dve"]
        STG = 2048

        def cvt(out_ap, in_ap, gain_ap, reads, writes):
            e = cvt_engs[pst["cvt"] % 2]
            pst["cvt"] += 1
            rd = list(reads) + ([Bgcol] if gain_ap is not None else [])
            if e == "act":
                if gain_ap is None:
                    T.op("act", lambda en: en.activation(out=out_ap, in_=in_ap, func=AF.Copy), reads=rd, writes=writes)
                else:
                    T.op("act", lambda en: en.activation(out=out_ap, in_=in_ap, func=AF.Copy, scale=gain_ap), reads=rd, writes=writes)
            else:
                if gain_ap is None:
                    T.op(e, lambda en: en.tensor_copy(out=out_ap, in_=in_ap), reads=rd, writes=writes)
                else:
                    T.op(e, lambda en: en.tensor_scalar(out=out_ap, in0=in_ap, scalar1=gain_ap, scalar2=None, op0=ALU.mult),
                         reads=rd, writes=writes)

        def gc(n, kc):
            return gcol[:, n, kc:kc + 1]

        class Stager:
            def __init__(self, ins_, outs_, q):
                self.ins, self.outs, self.q, self.i = ins_, outs_, q, 0

            def next(self):
                k = self.i; self.i += 1
                fin, Bin = self.ins[k % len(self.ins)]
                fout, Bout = self.outs[k % len(self.outs)]
                return fin, Bin, fout, Bout

        stg1 = Stager([(allr[i][:].bitcast(F32), Ballr[i]) for i in range(4)],
                      [(allr[i][:, 0:STG], Ballr[i]) for i in range(4, 8)], "sp")

        fin0 = allr[0][:].bitcast(F32)
        fin1 = allr[1][:].bitcast(F32)
        T.dma("sp", [(fin0[:, 0:2048].rearrange("p (g c d) -> p g c d", g=4, c=2),
                      pool_w_d.rearrange("g (c p) d -> p g c d", p=128))], writes=[Ballr[0]], sem_on=(Ballr[0], "w"))
        T.dma("sp", [(fin1[:, 0:1024], pool_scale_d.partition_broadcast(128))], writes=[Ballr[1]], sem_on=(Ballr[1], "w"))
        for g in range(4):
            for c in range(2):
                T.op("dve", lambda e, g=g, c=c: e.scalar_tensor_tensor(
                    out=wpool[:, g, c, :], in0=fin0[:, (g * 2 + c) * 256:(g * 2 + c + 1) * 256], scalar=gc(G_POOL, 2 * g + c),
                    in1=fin1[:, g * 256:(g + 1) * 256], op0=ALU.mult, op1=ALU.mult),
                    reads=[Ballr[0], Ballr[1], Bgcol], writes=[Bwpool])
        with nc.allow_non_contiguous_dma(reason="one-time 32KiB router weight load"):
            T.dma("sp", [(wrf[:], router_w_d.rearrange("(kc p) e -> p kc e", p=128))], writes=[Bwrf], sem_on=(Bwrf, "w"))
        T.op("dve", lambda e: e.tensor_tensor(out=wr[:], in0=wrf[:], in1=gcol[:, G_FFN1, :].unsqueeze(2).to_broadcast([128, 8, NE]), op=ALU.mult),
             reads=[Bwrf, Bgcol], writes=[Bwr])

        def pre_rows(stg, W2d, scr, Bscr, nrow_chunks, ncols, gain_idx):
            Wv = W2d.rearrange("(r p) n -> p r n", p=128)
            per = max(1, STG // ncols)
            r0 = 0
            while r0 < nrow_chunks:
                nr = min(per, nrow_chunks - r0)
                fin, Bin, fout, Bout = stg.next()
                yield (stg.q, [], [Bin])
                T.dma(stg.q, [(fin[:, 0:nr * ncols].rearrange("p (r n) -> p r n", r=nr), Wv[:, r0:r0 + nr, :])],
                      writes=[Bin], sem_on=(Bin, "w"))
                if gain_idx is None:
                    cvt(fout[:, 0:nr * ncols], fin[:, 0:nr * ncols], None, [Bin], [Bout])
                else:
                    for rr in range(nr):
                        cvt(fout[:, rr * ncols:(rr + 1) * ncols], fin[:, rr * ncols:(rr + 1) * ncols], gc(gain_idx, r0 + rr), [Bin], [Bout])
                T.dma("pool", [(scr[:, r0:r0 + nr, :], fout[:, 0:nr * ncols].rearrange("p (r n) -> p r n", r=nr))],
                      reads=[Bout], writes=[Bscr], sem_on=(Bout, "r"))
                r0 += nr

        def pre_gu(stg, W2d, nj, scr, Bscr, gain_idx):
            Wv = W2d.rearrange("(kc p) n -> p kc n", p=128)
            for kc in range(8):
                j0 = 0
                while j0 < nj:
                    jn = min(8, nj - j0)
                    w = jn * 128
                    fin, Bin, fout, Bout = stg.next()
                    yield (stg.q, [], [Bin])
                    T.dma(stg.q, [(fin[:, 0:w], Wv[:, kc, j0 * 128:j0 * 128 + w]),
                                  (fin[:, w:2 * w], Wv[:, kc, nj * 128 + j0 * 128:nj * 128 + j0 * 128 + w])],
                          writes=[Bin], sem_on=(Bin, "w"))
                    ov = fout[:, 0:2 * w].rearrange("p (j two c) -> p two j c", two=2, c=128)
                    iv = fin[:, 0:2 * w].rearrange("p (two j c) -> p two j c", two=2, c=128)
                    cvt(ov, iv, gc(gain_idx, kc), [Bin], [Bout])
                    T.dma("pool", [(scr[:, kc, j0 * 256:j0 * 256 + 2 * w], fout[:, 0:2 * w])],
                          reads=[Bout], writes=[Bscr], sem_on=(Bout, "r"))
                    j0 += jn

        def prepass1():
            if last_stage >= STAGES.index("ffn"):
                yield from pre_gu(stg1, w_gu_d, NJ, s_gu, B_s_gu, G_FFN0)
                yield from pre_rows(stg1, w_down_d, s_down, B_s_down, NJ, D, None)
            if last_stage >= STAGES.index("ple0"):
                yield from pre_rows(stg1, pgw_d[0], s_pg[0], B_s_pg[0], 8, D, G_PG0)
                yield from pre_rows(stg1, plew_d[0], s_plew[0], B_s_plew[0], 2, D, None)
            if last_stage >= STAGES.index("attn"):
                yield from pre_rows(stg1, w_kv_d, s_kv, B_s_kv, 8, 512, G_KV)
                yield from pre_rows(stg1, w_q_d, s_q, B_s_q, 8, D, G_Q)
                yield from pre_rows(stg1, w_o_d, s_o, B_s_o, 8, D, None)

        def prepass2(stg):
            if last_stage >= STAGES.index("moe"):
                for e_ in range(NE):
                    yield from pre_gu(stg, we_gu_d[e_], 8, s_egu[e_], B_s_egu[e_], G_FFN1)
                    yield from pre_rows(stg, we_down_d[e_], s_ed[e_], B_s_ed[e_], 8, D, None)
            if last_stage >= STAGES.index("ple1"):
                yield from pre_rows(stg, pgw_d[1], s_pg[1], B_s_pg[1], 8, D, G_PG1)
                yield from pre_rows(stg, plew_d[1], s_plew[1], B_s_plew[1], 2, D, None)

        for _ in prepass1():
            pass

        def P(reads, writes=(), eng="pe"):
            return (eng, list(reads), list(writes))

        def wsA(src_ap, Bsrc):
            i = pst["ringA"]; pst["ringA"] = (i + 1) % NRA
            n = 1
            for d_ in src_ap.shape[1:]:
                n *= d_
            assert n <= SLOT
            dst = ringA[i][:, 0:n]
            if len(src_ap.shape) == 3:
                dst = dst.rearrange("p (a b) -> p a b", a=src_ap.shape[1])
            T.dma("sp", [(dst, src_ap)], reads=[Bsrc], writes=[BringA[i]], sem_on=(BringA[i], "w"))
            return i

        def wsB(i, src_ap, Bsrc):
            n = 1
            for d_ in src_ap.shape[1:]:
                n *= d_
            dst = ringB[i][:, 0:n]
            if len(src_ap.shape) == 3:
                dst = dst.rearrange("p (a b) -> p a b", a=src_ap.shape[1])
            T.dma("sp", [(dst, src_ap)], reads=[Bsrc], writes=[BringB[i]], sem_on=(BringB[i], "w"))

        def norm_stats(hc, Bhc, ss, Bss, rstd, Brstd):
            for s in range(NSUB):
                if s % 2 == 0:
                    T.op("act", lambda e, s=s: e.activation(out=junk[:], in_=hc[:, s, :], func=AF.Square, accum_out=ss[:, s:s + 1]),
                         reads=[Bhc[s]], writes=[Bss], cost=1.3)
                else:
                    T.op("dve", lambda e, s=s: e.scalar_tensor_tensor(out=junk2[:], in0=hc[:, s, :], scalar=1.0, in1=hc[:, s, :], op0=ALU.mult, op1=ALU.mult,
                                                                      accum_out=ss[:, s:s + 1]),
                         reads=[Bhc[s]], writes=[Bss], cost=1.4)
            T.op("dve", lambda e: e.tensor_scalar(out=ss[:], in0=ss[:], scalar1=1.0 / D, scalar2=EPS, op0=ALU.mult, op1=ALU.add),
                 reads=[Bss], writes=[Bss], cost=0.2)
            T.op("act", lambda e: e.activation(out=ss[:], in_=ss[:], func=AF.Sqrt), reads=[Bss], writes=[Bss], cost=0.25, tbl='S')
            T.op("dve", lambda e: e.reciprocal(out=rstd[:], in_=ss[:]), reads=[Bss], writes=[Brstd], cost=0.2)

        BssP = [Buf("ssP%d" % i) for i in range(NSUB)]; BrstdP = [Buf("rstdP%d" % i) for i in range(NSUB)]

        def norm_stats1(hc, Bhc, s, ss, Bss_, rstd, Brstd_):
            sc = ss[:, s:s + 1]
            if s % 2 == 0:
                T.op("act", lambda e: e.activation(out=junk[:], in_=hc[:, s, :], func=AF.Square, accum_out=sc), reads=[Bhc[s]], writes=[Bss_[s]], cost=1.3)
            else:
                T.op("dve", lambda e: e.scalar_tensor_tensor(out=junk2[:], in0=hc[:, s, :], scalar=1.0, in1=hc[:, s, :], op0=ALU.mult, op1=ALU.mult,
                                                             accum_out=sc), reads=[Bhc[s]], writes=[Bss_[s]], cost=1.4)
            T.op("dve", lambda e: e.tensor_scalar(out=sc, in0=sc, scalar1=1.0 / D, scalar2=EPS, op0=ALU.mult, op1=ALU.add),
                 reads=[Bss_[s]], writes=[Bss_[s]], cost=0.2)
            T.op("act", lambda e: e.activation(out=sc, in_=sc, func=AF.Sqrt), reads=[Bss_[s]], writes=[Bss_[s]], cost=0.25, tbl='S')
            T.op("dve", lambda e: e.reciprocal(out=rstd[:, s:s + 1], in_=sc), reads=[Bss_[s]], writes=[Brstd_[s]], cost=0.2)

        def make_u(hc, Bhc, s, ub, rstd, Brstd):
            T.op("dve", lambda e: e.tensor_scalar(out=u[ub][:], in0=hc[:, s, :], scalar1=rstd[:, s:s + 1], scalar2=None, op0=ALU.mult),
                 reads=[Bhc[s], Brstd], writes=[Bu[ub]], cost=1.3)

        def transpose_to(k, src, Bsrc, nch, dst_view, Bdst):
            def f(e):
                for c in range(nch):
                    ins = e.transpose(psT[:, k, c * 128:(c + 1) * 128], src[:, c * 128:(c + 1) * 128], ident[:])
                return ins
            T.op("pe", f, reads=[Bsrc, Bident], writes=[BpT[k]], cost=0.08 * nch)
            pv = psT[:, k, 0:nch * 128].rearrange("p (a b) -> p a b", a=nch)
            T.op("act", lambda e: e.activation(out=dst_view, in_=pv, func=AF.Copy), reads=[BpT[k]], writes=[Bdst], cost=0.3 + 0.09 * nch)

        LBL = [""]

        def norm_sub(hc, Bhc, s, stream, ub0_=None):
            T.tag = stream + ":norm"
            ub0, uT_, BuT_, k = (0, uTa, BuTa, 0) if stream == "A" else (3, uTb, BuTb, 1)
            if ub0_ is not None:
                ub0 = ub0_
            ss, rstd, Bs, Br = ssN[stream], rstdN[stream], BssN[stream][s], BrstdN[stream][s]
            sc = ss[:, s:s + 1]
            if s % 2 == 0:
                T.op("act", lambda e: e.activation(out=junk[:], in_=hc[:, s, :], func=AF.Square, accum_out=sc), reads=[Bhc[s]], writes=[Bs], cost=1.3)
            else:
                T.op("dve", lambda e: e.scalar_tensor_tensor(out=junk2[:], in0=hc[:, s, :], scalar=1.0, in1=hc[:, s, :], op0=ALU.mult, op1=ALU.mult,
                                                             accum_out=sc), reads=[Bhc[s]], writes=[Bs], cost=1.4)
            T.op("dve", lambda e: e.tensor_scalar(out=sc, in0=sc, scalar1=1.0 / D, scalar2=EPS, op0=ALU.mult, op1=ALU.add), reads=[Bs], writes=[Bs], cost=0.2)
            T.op("act", lambda e: e.activation(out=sc, in_=sc, func=AF.Sqrt), reads=[Bs], writes=[Bs], cost=0.25, tbl='S')
            T.op("dve", lambda e: e.reciprocal(out=rstd[:, s:s + 1], in_=sc), reads=[Bs], writes=[Br], cost=0.2)
            ub = ub0 + s % 2
            T.op("dve", lambda e: e.tensor_scalar(out=u[ub][:], in0=hc[:, s, :], scalar1=rstd[:, s:s + 1], scalar2=None, op0=ALU.mult),
                 reads=[Bhc[s], Br], writes=[Bu[ub]], cost=1.3)

        def norm_T(s, stream, ub0_=None):
            T.tag = stream + ":norm:" + LBL[0]
            ub0, uT_, BuT_, k = (0, uTa, BuTa, 0) if stream == "A" else (3, uTb, BuTb, 1)
            if ub0_ is not None:
                ub0 = ub0_
            ub = ub0 + s % 2
            yield P([Bu[ub]], [BpT[k]])
            transpose_to(k, u[ub], Bu[ub], 8, uT_[:, :, s * 128:(s + 1) * 128], BuT_[s])

        def norm_to_uT(hc, Bhc, stream, label="first"):
            LBL[0] = label
            for s in range(NSUB):
                norm_sub(hc, Bhc, s, stream)
                yield from norm_T(s, stream)

        class LagNorm:
            def __init__(self, hc, Bhc, stream="A", label="lag", ub0=None):
                self.hc, self.Bhc, self.stream, self.pending, self.label, self.ub0 = hc, Bhc, stream, [], label, ub0

            def done(self, s, keep=1):
                norm_sub(self.hc, self.Bhc, s, self.stream, self.ub0)
                self.pending.append(s)
                yield from self.flush(keep)

            def flush(self, keep=0):
                LBL[0] = self.label
                while len(self.pending) > keep:
                    yield from norm_T(self.pending.pop(0), self.stream, self.ub0)

            def need(self, s):
                LBL[0] = self.label
                while self.pending and self.pending[0] <= s:
                    yield from norm_T(self.pending.pop(0), self.stream, self.ub0)

        def up_chunk(si, nch, jj, j, hb):
            sv = ringA[si][:, 0:8 * nch * 256].rearrange("p (k j c) -> p k j c", k=8, j=nch)
            ba = psA(); bg = psA()
            tb = pst["th"] % 2; pst["th"] += 1
            yield P([BringA[si]] + BuTa, [BpF[ba], BpF[bg]])
            T.tag = "A:up"

            def fa(e):
                for kc in range(8):
                    ins = e.matmul(psF[:, ba, :], lhsT=sv[:, kc, jj, 0:128], rhs=uTa[:, kc, :], start=(kc == 0), stop=(kc == 7))
                return ins

            def fg(e):
                for kc in range(8):
                    ins = e.matmul(psF[:, bg, :], lhsT=sv[:, kc, jj, 128:256], rhs=uTa[:, kc, :], start=(kc == 0), stop=(kc == 7))
                return ins
            T.op("pe", fa, reads=[BringA[si]] + BuTa, writes=[BpF[ba]], cost=1.75)
            T.op("pe", fg, reads=[BringA[si]] + BuTa, writes=[BpF[bg]], cost=1.75)
            T.op("act", lambda e: e.activation(out=th[tb][:], in_=psF[:, ba, :], func=AF.Tanh, scale=0.5), reads=[BpF[ba]], writes=[Bth[tb]], cost=0.65, tbl='E')
            T.op("dve", lambda e: e.scalar_tensor_tensor(out=mm[tb][:], in0=th[tb][:], scalar=1.0, in1=psF[:, bg, :], op0=ALU.add, op1=ALU.mult),
                 reads=[Bth[tb], BpF[bg]], writes=[Bmm[tb]])
            T.op("dve", lambda e: e.scalar_tensor_tensor(out=hm[hb][:, j, :], in0=psF[:, ba, :], scalar=0.5, in1=mm[tb][:], op0=ALU.mult, op1=ALU.mult),
                 reads=[BpF[ba], Bmm[tb]], writes=[Bhm[hb]])

        def down_units(hc, Bhc, down_src, Bdown_src, j0, nj, gate_e, hb, hook=None):
            pieces = []
            jx = 0
            while jx < nj:
                n = min(4, nj - jx)
                pieces.append((wsA(down_src[:, j0 + jx:j0 + jx + n, :], Bdown_src), n))
                jx += n
            for s in range(NSUB):
                for half in range(2):
                    b = psA()
                    yield P([Bhm[hb]] + [BringA[si] for si, _ in pieces], [BpF[b]])
                    T.tag = "A:down"

                    def fd(e, s=s, half=half, b=b):
                        jx = 0
                        for si, n in pieces:
                            dv = ringA[si][:, 0:n * D].rearrange("p (j n) -> p j n", j=n)
                            for q in range(n):
                                ins = e.matmul(psF[:, b, :], lhsT=hm[hb][:, jx, s * 128:(s + 1) * 128], rhs=dv[:, q, half * 512:(half + 1) * 512],
                                               start=(jx == 0), stop=(jx == nj - 1))
                                jx += 1
                        return ins
                    T.op("pe", fd, reads=[Bhm[hb]] + [BringA[si] for si, _ in pieces], writes=[BpF[b]], cost=0.216 * nj)
                    hv = hc[:, s, half * 512:(half + 1) * 512]
                    if gate_e is None:
                        T.op("dve", lambda e, b=b, hv=hv: e.tensor_tensor(out=hv, in0=hv, in1=psF[:, b, :], op=ALU.add),
                             reads=[BpF[b], Bhc[s]], writes=[Bhc[s]])
                    else:
                        T.op("dve", lambda e, b=b, hv=hv, s=s: e.scalar_tensor_tensor(out=hv, in0=psF[:, b, :], scalar=gates[:, s, gate_e:gate_e + 1],
                                                                                      in1=hv, op0=ALU.mult, op1=ALU.add),
                             reads=[BpF[b], Bhc[s], Bgates], writes=[Bhc[s]])
                if hook is not None:
                    yield from hook(s)

        def ffn_groups(hc, Bhc, groups, tail_hook=None, extra=None):
            pending = None
            for gi, (up_fn, Bup, down_src, Bdown, j0, nj, gate_e) in enumerate(groups):
                hb = gi % 2
                j = 0
                while j < nj:
                    nch = min(2, nj - j)
                    si = wsA(up_fn(j0 + j, nch), Bup)
                    for jj in range(nch):
                        yield from up_chunk(si, nch, jj, j, hb)
                        j += 1
                        if pending is not None:
                            yield from pending
                            pending = None
                pending = down_units(hc, Bhc, down_src, Bdown, j0, nj, gate_e, hb,
                                     hook=(tail_hook if gi == len(groups) - 1 else None))
                if gi == 0 and extra is not None:
                    yield from extra
            yield from pending

        def issue_p(l, t0):
            T.dma("pool", [(pb[:].rearrange("p (s f) -> p s f", s=NSUB), p_d[l][t0:t0 + TM, :].rearrange("(s p) f -> p s f", p=128))],
                  writes=[Bpb], sem_on=(Bpb, "w"))

        def load_p(l, t0, issued=False):
            if not issued:
                issue_p(l, t0)
            for s in range(NSUB):
                yield P([Bpb], [BpT[0]])
                T.tag = "A:pT"
                transpose_to(0, pb[:, s * 256:(s + 1) * 256], Bpb, 2, pT[:, :, s * 128:(s + 1) * 128], BpT_)

        def ple(hc, Bhc, l, t0, lag=None, tail_hook=None, p_loaded=False):
            if lag is None:
                yield from norm_to_uT(hc, Bhc, "A")
            if not p_loaded:
                yield from load_p(l, t0)
            iw = wsA(s_plew[l], B_s_plew[l])
            wv = ringA[iw][:, 0:2 * D].rearrange("p (k n) -> p k n", k=2)
            sis = [wsA(s_pg[l][:, :, half * 512:(half + 1) * 512], B_s_pg[l]) for half in range(2)]
            for s in range(NSUB):
                if lag is not None:
                    yield from lag.need(s)
                for half in range(2):
                    si = sis[half]
                    sv = ringA[si][:, 0:8 * 512].rearrange("p (k n) -> p k n", k=8)
                    bgt = psA(); be = psA()
                    tb = pst["th"] % 2; pst["th"] += 1
                    yield P([BuTa[s], BringA[si], BpT_, BringA[iw]], [BpF[bgt], BpF[be]])
                    T.tag = "A:ple"

                    def fgate(e, s=s, bgt=bgt, sv=sv):
                        for kc in range(8):
                            ins = e.matmul(psF[:, bgt, :], lhsT=uTa[:, kc, s * 128:(s + 1) * 128], rhs=sv[:, kc, :], start=(kc == 0), stop=(kc == 7))
                        return ins

                    def fe(e, s=s, half=half, be=be):
                        for kc in range(2):
                            ins = e.matmul(psF[:, be, :], lhsT=pT[:, kc, s * 128:(s + 1) * 128], rhs=wv[:, kc, half * 512:(half + 1) * 512],
                                           start=(kc == 0), stop=(kc == 1))
                        return ins
                    T.op("pe", fgate, reads=[BuTa[s], BringA[si]], writes=[BpF[bgt]], cost=1.75)
                    T.op("pe", fe, reads=[BpT_, BringA[iw]], writes=[BpF[be]], cost=0.45)
                    T.op("act", lambda e, bgt=bgt, tb=tb: e.activation(out=th[tb][:], in_=psF[:, bgt, :], func=AF.Tanh, scale=0.5),
                         reads=[BpF[bgt]], writes=[Bth[tb]], cost=0.65, tbl='E')
                    T.op("dve", lambda e, be=be, tb=tb: e.scalar_tensor_tensor(out=mm[tb][:], in0=th[tb][:], scalar=1.0, in1=psF[:, be, :],
                                                                               op0=ALU.add, op1=ALU.mult),
                         reads=[Bth[tb], BpF[be]], writes=[Bmm[tb]])
                    hv = hc[:, s, half * 512:(half + 1) * 512]
                    T.op("dve", lambda e, tb=tb, hv=hv: e.scalar_tensor_tensor(out=hv, in0=mm[tb][:], scalar=0.5, in1=hv, op0=ALU.mult, op1=ALU.add),
                         reads=[Bmm[tb], Bhc[s]], writes=[Bhc[s]])
                    if half == 1 and tail_hook is not None:
                        yield from tail_hook(s)

        def qk_norm_rope(xf, Bxf, H, tab0, s, out_bf, Bout):
            n = H * 64
            xv = xf[:, 0:n].rearrange("p (h d) -> p h d", h=H)
            T.op("act", lambda e: e.activation(out=rtt[:, 0:n], in_=xf[:, 0:n], func=AF.Square), reads=[Bxf], writes=Brt)
            T.op("dve", lambda e: e.tensor_reduce(out=ssq[:, 0:H], in_=rtt[:, 0:n].rearrange("p (h d) -> p h d", h=H), axis=AX.X, op=ALU.add),
                 reads=Brt, writes=[Bssq])
            T.op("dve", lambda e: e.tensor_scalar(out=ssq[:, 0:H], in0=ssq[:, 0:H], scalar1=1.0 / 64, scalar2=EPS, op0=ALU.mult, op1=ALU.add),
                 reads=[Bssq], writes=[Bssq], cost=0.2)
            T.op("act", lambda e: e.activation(out=ssq[:, 0:H], in_=ssq[:, 0:H], func=AF.Sqrt), reads=[Bssq], writes=[Bssq], cost=0.25, tbl='S')
            T.op("dve", lambda e: e.reciprocal(out=rq[:, 0:H], in_=ssq[:, 0:H]), reads=[Bssq], writes=[Brq], cost=0.25)
            T.op("dve", lambda e: e.tensor_tensor(out=xv, in0=xv, in1=rq[:, 0:H].unsqueeze(2).to_broadcast([128, H, 64]), op=ALU.mult),
                 reads=[Bxf, Brq], writes=[Bxf])
            x1 = xv[:, :, 0:32]; x2 = xv[:, :, 32:64]
            tb = lambda i: rtab[:, tab0 + i, s, :].unsqueeze(1).to_broadcast([128, H, 32])
            t1 = rtt[:, 0:H * 32].rearrange("p (h d) -> p h d", h=H)
            t2 = rtt[:, 512:512 + H * 32].rearrange("p (h d) -> p h d", h=H)
            ov = out_bf[:, 0:n].rearrange("p (h d) -> p h d", h=H)
            T.op("dve", lambda e: e.tensor_tensor(out=t1, in0=x1, in1=tb(0), op=ALU.mult), reads=[Bxf, Brtab], writes=[Brt[0]])
            T.op("pool", lambda e: e.tensor_tensor(out=t2, in0=x2, in1=tb(1), op=ALU.mult), reads=[Bxf, Brtab], writes=[Brt[1]])
            T.op("dve", lambda e: e.tensor_tensor(out=ov[:, :, 0:32], in0=t1, in1=t2, op=ALU.subtract), reads=Brt, writes=[Bout])
            T.op("dve", lambda e: e.tensor_tensor(out=t1, in0=x2, in1=tb(2), op=ALU.mult), reads=[Bxf, Brtab], writes=[Brt[0]])
            T.op("pool", lambda e: e.tensor_tensor(out=t2, in0=x1, in1=tb(3), op=ALU.mult), reads=[Bxf, Brtab], writes=[Brt[1]])
            T.op("dve", lambda e: e.tensor_tensor(out=ov[:, :, 32:64], in0=t1, in1=t2, op=ALU.add), reads=Brt, writes=[Bout])

        def att(m, hc, Bhc):
            t0 = m * TM
            wsB(0, s_kv, B_s_kv)
            wsB(1, s_q[:, :, 0:512], B_s_q)
            wsB(2, s_q[:, :, 512:1024], B_s_q)
            T.dma("sp", [(cs[:], cos_d[t0:t0 + TM, :].rearrange("(s p) f -> p s f", p=128))], writes=[Bcs], sem_on=(Bcs, "w"))
            T.dma("sp", [(sn[:], sin_d[t0:t0 + TM, :].rearrange("(s p) f -> p s f", p=128))], writes=[Bsn], sem_on=(Bsn, "w"))
            for qi, gbc in enumerate((gq_bc, gk_bc)):
                for ti, (tab, lo) in enumerate(((cs, 0), (sn, 32), (cs, 32), (sn, 0))):
                    T.op("pool", lambda e, qi=qi, ti=ti, tab=tab, lo=lo, gbc=gbc: e.tensor_tensor(
                        out=rtab[:, qi * 4 + ti, :, :], in0=tab[:], in1=gbc[:, lo:lo + 32].unsqueeze(1).to_broadcast([128, NSUB, 32]), op=ALU.mult),
                        reads=[Bcs, Bsn, Bgqk], writes=[Brtab])
            yield from norm_to_uT(hc, Bhc, "B")
            kvv = ringB[0][:, 0:8 * 512].rearrange("p (k n) -> p k n", k=8)
            qv = [ringB[1 + h_][:, 0:8 * 512].rearrange("p (k n) -> p k n", k=8) for h_ in range(2)]
            for s in range(NSUB):
                bkv = psB()
                yield P([BuTb[s], BringB[0]], [BpF[bkv]])
                T.tag = "B:kvproj"

                def fkv(e, s=s, bkv=bkv):
                    for kc in range(8):
                        ins = e.matmul(psF[:, bkv, :], lhsT=uTb[:, kc, s * 128:(s + 1) * 128], rhs=kvv[:, kc, :], start=(kc == 0), stop=(kc == 7))
                    return ins
                T.op("pe", fkv, reads=[BuTb[s], BringB[0]], writes=[BpF[bkv]], cost=1.75)
                T.op("act", lambda e, bkv=bkv: e.activation(out=kf[:], in_=psF[:, bkv, 0:256], func=AF.Copy), reads=[BpF[bkv]], writes=[Bkf])
                T.op("act", lambda e, bkv=bkv, s=s: e.activation(out=Vaug[:, 1 + s, :, 0:64], in_=psF[:, bkv, 256:512].rearrange("p (h d) -> p h d", h=4),
                                                                 func=AF.Copy), reads=[BpF[bkv]], writes=[BV[1 + s]])
                for half in range(2):
                    bq = psB()
                    yield P([BuTb[s], BringB[1 + half]], [BpF[bq]])
                    T.tag = "B:qproj"

                    def fq(e, s=s, half=half, bq=bq):
                        for kc in range(8):
                            ins = e.matmul(psF[:, bq, :], lhsT=uTb[:, kc, s * 128:(s + 1) * 128], rhs=qv[half][:, kc, :], start=(kc == 0), stop=(kc == 7))
                        return ins
                    T.op("pe", fq, reads=[BuTb[s], BringB[1 + half]], writes=[BpF[bq]], cost=1.75)
                    T.op("act", lambda e, half=half, bq=bq: e.activation(out=qf[:, half * 512:(half + 1) * 512], in_=psF[:, bq, :], func=AF.Copy),
                         reads=[BpF[bq]], writes=[Bqf])
                qk_norm_rope(kf, Bkf, 4, 4, s, kr, Bkr)
                yield P([Bkr], [BpT[1]])
                T.tag = "B:kT"
                transpose_to(1, kr, Bkr, 2, KT[:, :, (1 + s) * 128:(2 + s) * 128], BK[1 + s])
                qk_norm_rope(qf, Bqf, 16, 0, s, qr, Bqr)
                yield P([Bqr], [BpT[1]])
                T.tag = "B:qT"
                transpose_to(1, qr, Bqr, 8, QT[:, :, s * 128:(s + 1) * 128], BQT)
            wsB(1, s_o[:, :, 0:512], B_s_o)
            wsB(2, s_o[:, :, 512:1024], B_s_o)
            ovw = [ringB[1 + h_][:, 0:8 * 512].rearrange("p (k n) -> p k n", k=8) for h_ in range(2)]
            for b in range(NSUB):
                first = (m == 0 and b == 0)
                kbs = [1 + b] if first else [b, 1 + b]
                n = len(kbs)
                for hk in range(4):
                    r0 = 0 if hk % 2 == 0 else 64
                    kch = hk // 2
                    qc0 = 4 * (hk // 2)
                    pos0 = 8 * (hk // 2) + (hk % 2)
                    pb_ = pst["pt"] % 2; pst["pt"] += 1
                    for i, kb in enumerate(kbs):
                        ps = psB()
                        yield P([BK[kb], BQT], [BpF[ps], BPT[pb_]])
                        T.tag = "B:S"
                        T.op("pe", lambda e, ps=ps, kb=kb: e.matmul(psF[:, ps, :].rearrange("p (a q) -> p a q", a=4),
                                                                   lhsT=KT[r0:r0 + 64, kch, kb * 128:(kb + 1) * 128],
                                                                   rhs=QT[r0:r0 + 64, qc0:qc0 + 4, b * 128:(b + 1) * 128], start=True, stop=True),
                             reads=[BK[kb], BQT], writes=[BpF[ps]], cost=0.3)
                        T.op("act", lambda e, ps=ps, i=i: e.activation(out=PT[pb_][:, i * 512:(i + 1) * 512], in_=psF[:, ps, :], func=AF.Exp,
                                                                      bias=negc[:, 0:1], scale=0.125),
                             reads=[BpF[ps], Bnegc], writes=[BPT[pb_]], cost=0.65, tbl='E')
                    pv4 = PT[pb_][:, 0:n * 512].rearrange("p (a g q) -> p a g q", a=n, g=4)
                    mk = masks[:, 2 - n:2, :].unsqueeze(2).to_broadcast([128, n, 4, 128])
                    T.op("dve", lambda e, pv4=pv4, mk=mk: e.tensor_tensor(out=pv4, in0=pv4, in1=mk, op=ALU.mult), reads=[BPT[pb_], Bmasks], writes=[BPT[pb_]],
                         cost=0.7)
                    po = psB()
                    yield P([BPT[pb_]] + [BV[kb] for kb in kbs], [BpF[po]])
                    T.tag = "B:PV"

                    def fo(e, po=po, pb_=pb_, kbs=kbs, hk=hk, n=n):
                        for g in range(4):
                            for i, kb in enumerate(kbs):
                                ins = e.matmul(psF[:, po, g * 66:g * 66 + 65], lhsT=PT[pb_][:, i * 512 + g * 128:i * 512 + (g + 1) * 128],
                                               rhs=Vaug[:, kb, hk, 0:65], start=(i == 0), stop=(i == n - 1))
                        return ins
                    T.op("pe", fo, reads=[BPT[pb_]] + [BV[kb] for kb in kbs], writes=[BpF[po]], cost=0.07 * 4 * n)
                    pov = psF[:, po, 0:264].rearrange("p (g d) -> p g d", g=4)
                    T.op("dve", lambda e, pov=pov, pos0=pos0: e.tensor_tensor(out=den[:], in0=pov[:, :, 64], in1=esink[:, pos0:pos0 + 7:2], op=ALU.add),
                         reads=[BpF[po], Besink], writes=[Bden], cost=0.2)
                    T.op("dve", lambda e: e.reciprocal(out=rden[:], in_=den[:]), reads=[Bden], writes=[Brden], cost=0.2)
                    o16 = Ot[:].rearrange("p (h d) -> p h d", h=16)
                    T.op("dve", lambda e, pov=pov, pos0=pos0, o16=o16: e.tensor_tensor(out=o16[:, pos0:pos0 + 7:2, :], in0=pov[:, :, 0:64],
                                                                                      in1=rden[:].unsqueeze(2).to_broadcast([128, 4, 64]), op=ALU.mult),
                         reads=[BpF[po], Brden], writes=[BO], cost=0.4)
                yield P([BO], [BpT[1]])
                T.tag = "B:OT"
                transpose_to(1, Ot, BO, 8, OT[:], BOT)
                for half in range(2):
                    b2 = psB()
                    yield P([BOT, BringB[1 + half]], [BpF[b2]])
                    T.tag = "B:oproj"

                    def fop(e, half=half, b2=b2):
                        for kc in range(8):
                            ins = e.matmul(psF[:, b2, :], lhsT=OT[:, kc, :], rhs=ovw[half][:, kc, :], start=(kc == 0), stop=(kc == 7))
                        return ins
                    T.op("pe", fop, reads=[BOT, BringB[1 + half]], writes=[BpF[b2]], cost=1.75)
                    hv = hc[:, b, half * 512:(half + 1) * 512]
                    T.op("dve", lambda e, b2=b2, hv=hv: e.tensor_tensor(out=hv, in0=hv, in1=psF[:, b2, :], op=ALU.add), reads=[BpF[b2], Bhc[b]], writes=[Bhc[b]])
            T.op("pool", lambda e: e.tensor_copy(out=KT[:, :, 0:128], in_=KT[:, :, 512:640]), reads=[BK[4]], writes=[BK[0]])
            T.op("pool", lambda e: e.tensor_copy(out=Vaug[:, 0, :, :], in_=Vaug[:, 4, :, :]), reads=[BV[4]], writes=[BV[0]])

        def moe(m, hc, Bhc, next_x=None):
            t0 = m * TM
            do_ple1_pre = last_stage >= STAGES.index("ple1") and last_stage >= STAGES.index("moe")
            if last_stage >= STAGES.index("moe"):
                yield from norm_to_uT(hc, Bhc, "A")
                bl = psA()
                yield P(BuTa, [BpF[bl]])
                T.tag = "A:router"

                def fl(e):
                    for s in range(NSUB):
                        for kc in range(8):
                            ins = e.matmul(psF[:, bl, s * 8:(s + 1) * 8], lhsT=uTa[:, kc, s * 128:(s + 1) * 128], rhs=wr[:, kc, :], start=(kc == 0), stop=(kc == 7))
                    return ins
                T.op("pe", fl, reads=BuTa + [Bwr], writes=[BpF[bl]], cost=2.0)
                T.op("dve", lambda e: e.tensor_tensor(out=Lg[:], in0=psF[:, bl, 0:32].rearrange("p (s e) -> p s e", s=NSUB),
                                                      in1=rb_bc[:].unsqueeze(1).to_broadcast([128, NSUB, NE]), op=ALU.add), reads=[BpF[bl], Brb], writes=[BLg])
                for s in range(NSUB):
                    T.op("dve", lambda e, s=s: e.max(out=m8[:, s, :], in_=Lg[:, s, :]), reads=[BLg], writes=[Bm8])
                T.op("dve", lambda e: e.tensor_scalar(out=nv1[:], in0=m8[:, :, 0], scalar1=-1.0, scalar2=None, op0=ALU.mult), reads=[Bm8], writes=[Bnv1])
                for s in range(NSUB):
                    T.op("act", lambda e, s=s: e.activation(out=ex[:, s, :], in_=Lg[:, s, :], func=AF.Exp, bias=nv1[:, s:s + 1]), reads=[BLg, Bnv1], writes=[Bex], cost=0.25, tbl='E')
                    T.op("dve", lambda e, s=s: e.tensor_scalar(out=msk[:, s, :], in0=Lg[:, s, :], scalar1=m8[:, s, 1:2], scalar2=None, op0=ALU.is_ge),
                         reads=[BLg, Bm8], writes=[Bmsk])
                T.op("dve", lambda e: e.tensor_tensor(out=ex[:], in0=ex[:], in1=msk[:], op=ALU.mult), reads=[Bex, Bmsk], writes=[Bex])
                T.op("dve", lambda e: e.tensor_reduce(out=gden[:], in_=ex[:], axis=AX.X, op=ALU.add), reads=[Bex], writes=[Bgden])
                T.op("dve", lambda e: e.reciprocal(out=gden[:], in_=gden[:]), reads=[Bgden], writes=[Bgden])
                T.op("dve", lambda e: e.tensor_tensor(out=gates[:], in0=ex[:], in1=gden[:].unsqueeze(2).to_broadcast([128, NSUB, NE]), op=ALU.mult),
                     reads=[Bex, Bgden], writes=[Bgates])
                if do_ple1_pre:
                    issue_p(1, t0)
                    pl = load_p(1, t0, issued=True)
                groups = [((lambda j, n, e_=e_: s_egu[e_][:, :, j * 256:(j + n) * 256]), B_s_egu[e_], s_ed[e_], B_s_ed[e_], 0, 8, e_) for e_ in range(NE)]
                do_ple1 = last_stage >= STAGES.index("ple1")
                ln = LagNorm(hc, Bhc, label="ple1")
                yield from ffn_groups(hc, Bhc, groups, tail_hook=ln.done if do_ple1 else None, extra=(pl if do_ple1_pre else None))

            ln = None if last_stage < STAGES.index("moe") else ln

            def finish(s):
                T.dma("pool", [(out_d[t0 + s * 128:t0 + (s + 1) * 128, :], hc[:, s, :])], reads=[Bhc[s]], sem_on=(Bhc[s], "r"))
                if next_x is not None:
                    t1 = next_x * TM
                    T.dma("sp", [(hc[:, s, :], x_d[t1 + s * 128:t1 + (s + 1) * 128, :])], writes=[Bhc[s]], sem_on=(Bhc[s], "w"))
                if False:
                    yield
            if last_stage >= STAGES.index("ple1"):
                yield from ple(hc, Bhc, 1, t0, lag=(ln if last_stage >= STAGES.index("moe") else None), tail_hook=finish, p_loaded=do_ple1_pre)
            else:
                for s in range(NSUB):
                    yield from finish(s)

        def layer0(m, hc, Bhc, x_loaded=False):
            t0 = m * TM
            do_ffn = last_stage >= STAGES.index("ffn")
            do_ple0 = last_stage >= STAGES.index("ple0")
            if not x_loaded:
                for s in range(NSUB):
                    T.dma("sp", [(hc[:, s, :], x_d[t0 + s * 128:t0 + (s + 1) * 128, :])], writes=[Bhc[s]], sem_on=(Bhc[s], "w"))
            lnp = LagNorm(hc, Bhc, label="ffn", ub0=5)
            if last_stage >= STAGES.index("pool"):
                for s in range(NSUB):
                    norm_stats1(hc, Bhc, s, ssA, BssP, rstdA, BrstdP)
                make_u(hc, Bhc, 0, 0, rstdA, BrstdP[0])
                for s in range(NSUB):
                    cur = s % 2
                    prev = 2 if s == 0 else (s - 1) % 2
                    first = (m == 0 and s == 0)
                    pd = psA_pair()
                    pdv = psF[:, pd:pd + 2, :].rearrange("p a (c t) -> p (a c) t", t=128)
                    yield P([Bu[cur]] + ([] if first else [Bu[prev]]), [BpF[pd], BpF[pd + 1]])
                    T.tag = "A:pool:d"

                    def fpool(e, cur=cur, prev=prev, first=first, pdv=pdv):
                        for c in range(8):
                            g = c // 2
                            if first:
                                ins = e.matmul(pdv[:, c, :], lhsT=u[cur][:, c * 128:(c + 1) * 128], rhs=poolA[:, 8 + g, :], start=True, stop=True)
                            else:
                                e.matmul(pdv[:, c, :], lhsT=u[cur][:, c * 128:(c + 1) * 128], rhs=poolA[:, g, :], start=True, stop=False)
                                ins = e.matmul(pdv[:, c, :], lhsT=u[prev][:, c * 128:(c + 1) * 128], rhs=poolA[:, 4 + g, :], start=False, stop=True)
                        return ins
                    T.op("pe", fpool, reads=[Bu[cur], BpoolA] + ([] if first else [Bu[prev]]), writes=[BpF[pd], BpF[pd + 1]], cost=1.4)
                    if s + 1 < NSUB:
                        make_u(hc, Bhc, s + 1, (s + 1) % 2, rstdA, BrstdP[s + 1])
                    T.op("act", lambda e, pdv=pdv: e.activation(out=dT[:], in_=pdv, func=AF.Copy), reads=[BpF[pd], BpF[pd + 1]], writes=[BdT], cost=1.2)
                    py = psA_pair()
                    yield P([BdT], [BpF[py], BpF[py + 1]])
                    T.tag = "A:pool:y"

                    def fy(e, py=py):
                        for g in range(4):
                            for cc in range(2):
                                ins = e.matmul(psF[:, py + g // 2, (g % 2) * 256:(g % 2 + 1) * 256], lhsT=dT[:, 2 * g + cc, :], rhs=wpool[:, g, cc, :],
                                               start=(cc == 0), stop=(cc == 1))
                        return ins
                    T.op("pe", fy, reads=[BdT, Bwpool], writes=[BpF[py], BpF[py + 1]], cost=1.2)
                    T.op("dve", lambda e, s=s, py=py: e.tensor_tensor(out=hc[:, s, :].rearrange("p (a n) -> p a n", a=2), in0=hc[:, s, :].rearrange("p (a n) -> p a n", a=2),
                                                                      in1=psF[:, py:py + 2, :], op=ALU.add),
                         reads=[BpF[py], BpF[py + 1], Bhc[s]], writes=[Bhc[s]], cost=1.4)
                    if do_ffn and s >= 1:
                        yield from lnp.done(s - 1, keep=1)
                T.op("pool", lambda e: e.tensor_copy(out=u[2][:], in_=u[1][:]), reads=[Bu[1]], writes=[Bu[2]])
                if do_ffn:
                    yield from lnp.done(NSUB - 1, keep=0)
            if do_ffn:
                if last_stage < STAGES.index("pool"):
                    yield from norm_to_uT(hc, Bhc, "A")
                if do_ple0:
                    issue_p(0, t0)
                    pl = load_p(0, t0, issued=True)
                groups = [((lambda j, n: s_gu[:, :, j * 256:(j + n) * 256]), B_s_gu, s_down, B_s_down, j0, nj, None) for (j0, nj) in ((0, 8), (8, 8), (16, 6))]
                ln0 = LagNorm(hc, Bhc, label="ple0")
                yield from ffn_groups(hc, Bhc, groups, tail_hook=ln0.done if do_ple0 else None, extra=(pl if do_ple0 else None))
            if do_ple0:
                yield from ple(hc, Bhc, 0, t0, lag=(ln0 if do_ffn else None), p_loaded=do_ffn)
            if False:
                yield

        def run(gen):
            for _ in gen:
                pass

        def chain(*gens):
            for g_ in gens:
                yield from g_

        def merge(gb, ga):
            def nxt(g_):
                try:
                    return next(g_)
                except StopIteration:
                    return None

            def est(p):
                eng, rd, wr = p
                return max(T.clock[eng], T.ready_time(rd, wr) + 0.1)
            pa = nxt(ga); pb = nxt(gb)
            while pa is not None or pb is not None:
                if pb is None:
                    pa = nxt(ga)
                elif pa is None:
                    pb = nxt(gb)
                elif est(pb) <= est(pa) + BIAS:
                    pb = nxt(gb)
                else:
                    pa = nxt(ga)

        ctx = lambda m: (hT[m % 2], Bh[m % 2])
        do_att = last_stage >= STAGES.index("attn")
        if not do_att:
            run(prepass2(stg1))
            for m in range(n_macro):
                run(layer0(m, *ctx(m)))
                run(moe(m, *ctx(m)))
        else:
            Bo = [Buf("c_out%d" % i) for i in range(4)]
            uTb_flat = uTb[:].rearrange("p a b -> p (a b)")
            stg2 = Stager([(ringB[0][:].bitcast(F32), BringB[0]), (ringB[1][:].bitcast(F32), BringB[1]),
                           (QT[:].rearrange("p a b -> p (a b)").bitcast(F32), BQT)],
                          [(ringB[2][:, 0:STG], Bo[0]), (ringB[2][:, STG:2 * STG], Bo[1]), (uTb_flat[:, 0:STG], Bo[2]), (uTb_flat[:, STG:2 * STG], Bo[3])],
                          "sp")
            for b_ in Bo[0:2]:
                T.absorb(b_, [BringB[2]])
            merge(prepass2(stg2), layer0(0, *ctx(0)))
            T.absorb(BringB[2], Bo[0:2])
            for b_ in BuTb:
                T.absorb(b_, Bo[2:4])
            for m in range(n_macro):
                a_parts = []
                if m >= 1:
                    a_parts.append(moe(m - 1, *ctx(m - 1), next_x=(m + 1 if m + 1 < n_macro else None)))
                if m + 1 < n_macro:
                    a_parts.append(layer0(m + 1, *ctx(m + 1), x_loaded=(m >= 1)))
                merge(att(m, *ctx(m)), chain(*a_parts))
                SIMLOG.append((m, dict(T.clock), dict(T.gaps)))
            run(moe(n_macro - 1, *ctx(n_macro - 1)))
        T.wait_all("pool", Bh[0] + Bh[1])
        build.stats = (T.ninst, T.nwait, T.nsem, nc.sbuf_bytes_remaining)
        build.sim = dict(T.clock)
        build.busy = dict(T.busy)
        build.gaps = dict(T.gaps)
    return nc


def _consts(seq):
    ident = np.eye(128, dtype=np.float32).astype(ml_dtypes.bfloat16)
    A = np.zeros((12, 128, 128), np.float32)
    tp = np.arange(128)[:, None]
    t = np.arange(128)[None, :]
    for g, win in enumerate((2, 4, 8, 16)):
        A[g] = np.where((tp <= t) & (tp > t - win), 1.0 / win, 0.0) - (tp == t)
        A[4 + g] = np.where(tp - 128 > t - win, 1.0 / win, 0.0)
        cnt = np.minimum(t + 1, win).astype(np.float32)
        A[8 + g] = np.where((tp <= t) & (tp > t - win), 1.0 / cnt, 0.0) - (tp == t)
    masks = np.zeros((128, 2, 128), np.float32)
    masks[:, 0, :] = (tp > t)
    masks[:, 1, :] = (tp <= t)
    inv = (10000.0 ** (-np.arange(0, 64, 2, dtype=np.float32) / 64)).astype(np.float32)
    ang = np.arange(seq, dtype=np.float32)[:, None] * inv[None, :]
    return {
        "ident": ident,
        "poolA": A.astype(ml_dtypes.bfloat16),
        "masks": masks.reshape(128, 256).astype(ml_dtypes.bfloat16),
        "cos": np.cos(ang).astype(np.float32),
        "sin": np.sin(ang).astype(np.float32),
    }


def make_in_maps(inputs, cores, seq):
    f = lambda a: np.ascontiguousarray(np.asarray(a, dtype=np.float32))
    cols = np.concatenate([np.arange(64) + 64 * h for h in PERM])
    shared = {
        "pool_norm": f(inputs["pool_norm"][0]), "pool_w": f(inputs["pool_w"][0]), "pool_scale": f(inputs["pool_scale"][0]),
        "kv_norm": f(inputs["kv_norm"]), "w_kv": f(inputs["w_kv"]), "k_norm": f(inputs["k_norm"]),
        "attn_norm": f(inputs["attn_norm"][0]), "w_q": f(np.asarray(inputs["w_q"][0])[:, cols]), "q_norm": f(inputs["q_norm"][0]),
        "sinks": f(np.asarray(inputs["sinks"][0])[PERM]), "w_o": f(np.asarray(inputs["w_o"][0])[cols, :]),
        "ffn_norm": f(inputs["ffn_norm"]), "w_gu": f(inputs["w_gu"][0]), "w_down": f(inputs["w_down"][0]),
        "router_w": f(inputs["router_w"][0]), "router_b": f(inputs["router_b"][0]),
        "we_gu": f(inputs["we_gu"][0]), "we_down": f(inputs["we_down"][0]),
        "ple_gate_norm": f(inputs["ple_gate_norm"]), "ple_gate_w": f(inputs["ple_gate_w"]), "ple_w": f(inputs["ple_w"]),
    }
    shared.update(_consts(seq))
    x = np.asarray(inputs["x"]); p = np.asarray(inputs["p"])
    maps = []
    for c in cores:
        d = dict(shared)
        d["x"] = f(x[c, :seq]); d["p0"] = f(p[0, c, :seq]); d["p1"] = f(p[1, c, :seq])
        maps.append(d)
    return maps


_NC_CACHE = {}


def kernel(**inputs):
    if "full" not in _NC_CACHE:
        _NC_CACHE["full"] = build()
    nc = _NC_CACHE["full"]
    in_maps = make_in_maps(inputs, list(range(8)), SEQ)
    res = run_bass_kernel_spmd(nc, in_maps, core_ids=list(range(8)))
    return np.stack([np.asarray(r["out"], dtype=np.float32) for r in res.results], axis=0)
```

```python
from contextlib import ExitStack

import ml_dtypes
import numpy as np

import concourse.bass as bass
import concourse.mybir as mybir
from concourse.bass_utils import run_bass_kernel_spmd

F32 = mybir.dt.float32
BF16 = mybir.dt.bfloat16
AF = mybir.ActivationFunctionType
ALU = mybir.AluOpType
AX = mybir.AxisListType

D = 1024
SEQ = 4096
TM = 512
NSUB = 4
DFF = 2816
NJ = DFF // 128
NE = 8
EPS = 1e-6
DEBUG = False
SIMLOG = []
REMAP = {}
SLOT = 4096
BIAS = 0.3

PERM = []
for _c in range(8):
    for _half in range(2):
        if _c < 4:
            PERM.append(_c + 4 * _half)
        else:
            PERM.append(8 + (_c - 4) + 4 * _half)


class Buf:
    __slots__ = ("name", "w", "r", "wsem", "wcnt", "rsem", "rcnt", "multi")

    def __init__(self, name, multi=False):
        self.name = name
        self.w = {}
        self.r = {}
        self.wsem = None
        self.wcnt = 0
        self.rsem = None
        self.rcnt = 0
        self.multi = multi


class Tracker:
    def __init__(self, nc, stack, same_engine_sync=True):
        self.nc = nc
        self.stack = stack
        self.same = same_engine_sync
        self.engs = {"pe": nc.tensor, "act": nc.scalar, "dve": nc.vector, "pool": nc.gpsimd, "sp": nc.sync}
        self.sem = {}
        self.seq = {}
        self.waited = {k: {} for k in self.engs}
        self.nsem = 0
        for k in ("pe", "act", "dve", "pool"):
            self.sem[k] = self.new_sem("e_" + k)
            self.seq[k] = 0
        self.ninst = 0
        self.nwait = 0
        self.remap = dict(REMAP)
        self.clock = {k: 0.0 for k in self.engs}
        self.tt = {}
        self.dma_free = 0.0
        self.busy = {}
        self.gaps = {}
        self.act_tbl = None
        self.tag = ''

    def new_sem(self, name):
        self.nsem += 1
        return self.stack.enter_context(self.nc.semaphore("%s_%d" % (name, self.nsem)))

    def _wait(self, e, tok):
        sem, val = tok
        own = self.sem.get(e)
        if own is not None and sem is own and not self.same:
            return
        key = id(sem)
        if self.waited[e].get(key, 0) >= val:
            return
        self.engs[e].wait_ge(sem, val)
        self.waited[e][key] = val
        self.nwait += 1

    def _deps(self, e, reads, writes):
        for b in reads:
            for tok in b.w.values():
                self._wait(e, tok)
        for b in writes:
            if not b.multi:
                for tok in b.w.values():
                    self._wait(e, tok)
            for tok in b.r.values():
                self._wait(e, tok)

    def _finish(self, reads, writes, tok):
        key = id(tok[0])
        for b in reads:
            old = b.r.get(key)
            if old is None or old[1] < tok[1]:
                b.r[key] = tok
        for b in writes:
            if b.multi:
                old = b.w.get(key)
                if old is None or old[1] < tok[1]:
                    b.w[key] = tok
            else:
                b.w = {key: tok}
                b.r = {}
        self.ninst += 1

    COST = {"pe": 0.3, "act": 0.8, "dve": 0.75, "pool": 1.2}

    def ready_time(self, reads, writes):
        t = 0.0
        for b in reads:
            for tok in b.w.values():
                t = max(t, self.tt.get((id(tok[0]), tok[1]), 0.0))
        for b in writes:
            for tok in list(b.w.values()) + list(b.r.values()):
                t = max(t, self.tt.get((id(tok[0]), tok[1]), 0.0))
        return t

    def op(self, e, fn, reads=(), writes=(), cost=None, tbl=None):
        e = self.remap.get(e, e)
        if tbl is not None and tbl != self.act_tbl:
            cost = (self.COST[e] if cost is None else cost) + 1.28
            self.act_tbl = tbl
        rt = self.ready_time(reads, writes)
        self._deps(e, reads, writes)
        ins = fn(self.engs[e])
        self.seq[e] += 1
        ins.then_inc(self.sem[e], 1)
        tok = (self.sem[e], self.seq[e])
        start = max(self.clock[e], rt + 0.1)
        if e == "pe" and start > self.clock[e]:
            self.gaps[self.tag] = self.gaps.get(self.tag, 0.0) + (start - self.clock[e])
        end = start + (self.COST[e] if cost is None else cost)
        self.busy[e] = self.busy.get(e, 0.0) + (end - start)
        self.clock[e] = end
        self.tt[(id(tok[0]), tok[1])] = end + (0.2 if e == "pe" else 0.05)
        self._finish(reads, writes, tok)
        return tok

    def dma(self, q, outs_ins, reads=(), writes=(), sem_on=None, **kw):
        rt = self.ready_time(reads, writes)
        self._deps(q, reads, writes)
        b, kind = sem_on
        tok = None
        issue = max(self.clock[q], rt + 0.1)
        nbytes = 0
        for out_ap, in_ap in outs_ins:
            nbytes += out_ap.nbytes() if callable(getattr(out_ap, "nbytes", None)) else 0
            issue += 0.6
            ins = self.engs[q].dma_start(out=out_ap, in_=in_ap, **kw)
            if kind == "w":
                if b.wsem is None:
                    b.wsem = self.new_sem("w_" + b.name)
                b.wcnt += 16
                ins.then_inc(b.wsem, 16)
                tok = (b.wsem, b.wcnt)
            else:
                if b.rsem is None:
                    b.rsem = self.new_sem("r_" + b.name)
                b.rcnt += 16
                ins.then_inc(b.rsem, 16)
                tok = (b.rsem, b.rcnt)
        self.clock[q] = issue
        done = max(issue + 2.0, self.dma_free) + nbytes / 3.0e5
        self.dma_free = done
        self.tt[(id(tok[0]), tok[1])] = done
        self._finish(reads, writes, tok)
        return tok

    def absorb(self, dst, srcs):
        for b in srcs:
            for tok in list(b.w.values()) + list(b.r.values()):
                key = id(tok[0])
                old = dst.r.get(key)
                if old is None or old[1] < tok[1]:
                    dst.r[key] = tok

    def wait_all(self, e, bufs):
        for b in bufs:
            for tok in b.w.values():
                self._wait(e, tok)
            for tok in b.r.values():
                self._wait(e, tok)


def build(n_macro=SEQ // TM, stop_after="ple1", same_engine_sync=True):
    STAGES = ["load", "pool", "ffn", "ple0", "attn", "moe", "ple1"]
    last_stage = STAGES.index(stop_after)
    seq = n_macro * TM
    nc = bass.Bass("TRN2", target_bir_lowering=False)

    def din(name, shape, dt=F32):
        return nc.dram_tensor(name, list(shape), dt, kind="ExternalInput").ap()

    x_d = din("x", [seq, D])
    p_d = [din("p0", [seq, 256]), din("p1", [seq, 256])]
    cos_d = din("cos", [seq, 32])
    sin_d = din("sin", [seq, 32])
    ident_d = din("ident", [128, 128], BF16)
    poolA_d = din("poolA", [12, 128, 128], BF16)
    masks_d = din("masks", [128, 256], BF16)
    pool_norm_d = din("pool_norm", [D])
    pool_w_d = din("pool_w", [4, 256, 256])
    pool_scale_d = din("pool_scale", [D])
    kv_norm_d = din("kv_norm", [D])
    w_kv_d = din("w_kv", [D, 512])
    k_norm_d = din("k_norm", [64])
    attn_norm_d = din("attn_norm", [D])
    w_q_d = din("w_q", [D, D])
    q_norm_d = din("q_norm", [64])
    sinks_d = din("sinks", [16])
    w_o_d = din("w_o", [D, D])
    ffn_norm_d = din("ffn_norm", [2, D])
    w_gu_d = din("w_gu", [D, 2 * DFF])
    w_down_d = din("w_down", [DFF, D])
    router_w_d = din("router_w", [D, NE])
    router_b_d = din("router_b", [NE])
    we_gu_d = din("we_gu", [NE, D, 2 * D])
    we_down_d = din("we_down", [NE, D, D])
    pgn_d = din("ple_gate_norm", [2, D])
    pgw_d = din("ple_gate_w", [2, D, D])
    plew_d = din("ple_w", [2, 256, D])
    out_d = nc.dram_tensor("out", [seq, D], F32, kind="ExternalOutput").ap()

    def dscr(name, shape):
        return nc.dram_tensor(name, list(shape), BF16, kind="Internal").ap()

    s_gu = dscr("s_gu", [128, 8, NJ * 256]); B_s_gu = Buf("s_gu", multi=True)
    s_down = dscr("s_down", [128, NJ, D]); B_s_down = Buf("s_down", multi=True)
    s_pg = [dscr("s_pg%d" % l, [128, 8, D]) for l in range(2)]; B_s_pg = [Buf("s_pg%d" % l, multi=True) for l in range(2)]
    s_kv = dscr("s_kv", [128, 8, 512]); B_s_kv = Buf("s_kv", multi=True)
    s_q = dscr("s_q", [128, 8, D]); B_s_q = Buf("s_q", multi=True)
    s_o = dscr("s_o", [128, 8, D]); B_s_o = Buf("s_o", multi=True)
    s_egu = [dscr("s_egu%d" % e, [128, 8, 2 * D]) for e in range(NE)]; B_s_egu = [Buf("s_egu%d" % e, multi=True) for e in range(NE)]
    s_ed = [dscr("s_ed%d" % e, [128, 8, D]) for e in range(NE)]; B_s_ed = [Buf("s_ed%d" % e, multi=True) for e in range(NE)]

    s_plew = [dscr("s_plew%d" % l, [128, 2, D]) for l in range(2)]; B_s_plew = [Buf("s_plew%d" % l, multi=True) for l in range(2)]

    with ExitStack() as st:
        T = Tracker(nc, st, same_engine_sync)

        def sb(name, shape, dt):
            return st.enter_context(nc.sbuf_tensor("sb_" + name, list(shape), dt))

        NRA, NRB = 5, 3
        ringA = [sb("ringA%d" % i, [128, SLOT], BF16) for i in range(NRA)]; BringA = [Buf("ringA%d" % i) for i in range(NRA)]
        ringB = [sb("ringB%d" % i, [128, SLOT], BF16) for i in range(NRB)]; BringB = [Buf("ringB%d" % i) for i in range(NRB)]
        hT = [sb("h%d" % c, [128, NSUB, D], F32) for c in range(2)]
        Bh = [[Buf("h%d_%d" % (c, s)) for s in range(NSUB)] for c in range(2)]
        u = [sb("u%d" % i, [128, D], BF16) for i in range(7)]; Bu = [Buf("u%d" % i) for i in range(7)]
        uTa = sb("uTa", [128, 8, TM], BF16); BuTa = [Buf("uTa%d" % i) for i in range(NSUB)]
        uTb = sb("uTb", [128, 8, TM], BF16); BuTb = [Buf("uTb%d" % i) for i in range(NSUB)]
        QT = sb("QT", [128, 8, TM], BF16); BQT = Buf("QT")
        hm = [sb("hm%d" % i, [128, 8, TM], BF16) for i in range(2)]; Bhm = [Buf("hm%d" % i) for i in range(2)]
        dT = sb("dT", [128, 8, 128], BF16); BdT = Buf("dT")
        junk = sb("junk", [128, D], BF16)
        junk2 = sb("junk2", [128, D], BF16)
        th = [sb("th%d" % i, [128, TM], F32) for i in range(2)]; Bth = [Buf("th%d" % i) for i in range(2)]
        mm = [sb("mm%d" % i, [128, TM], F32) for i in range(2)]; Bmm = [Buf("mm%d" % i) for i in range(2)]
        pb = sb("pb", [128, NSUB * 256], BF16); Bpb = Buf("pb")
        pT = sb("pT", [128, 2, TM], BF16); BpT_ = Buf("pT")
        qf = sb("qf", [128, D], F32); Bqf = Buf("qf")
        rtt = sb("rtt", [128, D], F32); Brt = [Buf("rt0"), Buf("rt1")]
        kf = sb("kf", [128, 256], F32); Bkf = Buf("kf")
        qr = sb("qr", [128, D], BF16); Bqr = Buf("qr")
        kr = sb("kr", [128, 256], BF16); Bkr = Buf("kr")
        KT = sb("KT", [128, 2, 5 * 128], BF16); BK = [Buf("K%d" % i) for i in range(5)]
        Vaug = sb("Vaug", [128, 5, 4, 66], BF16); BV = [Buf("V%d" % i) for i in range(5)]
        PT = [sb("PT%d" % i, [128, 1024], BF16) for i in range(2)]; BPT = [Buf("PT%d" % i) for i in range(2)]
        Ot = sb("Ot", [128, D], BF16); BO = Buf("Ot")
        OT = sb("OT", [128, 8, 128], BF16); BOT = Buf("OT")
        ident = sb("ident", [128, 128], BF16); Bident = Buf("ident")
        poolA = sb("poolA", [128, 12, 128], BF16); BpoolA = Buf("poolA")
        masks = sb("masks", [128, 2, 128], BF16); Bmasks = Buf("masks")
        wpool = sb("wpool", [128, 4, 2, 256], BF16); Bwpool = Buf("wpool")
        wr = sb("wr", [128, 8, NE], BF16); Bwr = Buf("wr")
        wrf = sb("wrf", [128, 8, NE], F32); Bwrf = Buf("wrf")
        gcol = sb("gcol", [128, 7, 8], F32); Bgcol = Buf("gcol")
        gq_bc = sb("gq_bc", [128, 64], F32); gk_bc = sb("gk_bc", [128, 64], F32); Bgqk = Buf("gqk")
        rb_bc = sb("rb_bc", [128, NE], F32); Brb = Buf("rb")
        sinks_bc = sb("sinks_bc", [128, 16], F32); Bsinks = Buf("sinks")
        esink = sb("esink", [128, 16], F32); Besink = Buf("esink")
        negc = sb("negc", [128, 1], F32); Bnegc = Buf("negc")
        mqk = sb("mqk", [128, 2], F32); Bmqk = Buf("mqk")
        cs = sb("cs", [128, NSUB, 32], F32); sn = sb("sn", [128, NSUB, 32], F32); Bcs = Buf("cs"); Bsn = Buf("sn")
        rtab = sb("rtab", [128, 8, NSUB, 32], F32); Brtab = Buf("rtab")
        ssA = sb("ssA", [128, NSUB], F32); BssA = Buf("ssA"); rstdA = sb("rstdA", [128, NSUB], F32); BrstdA = Buf("rstdA")
        ssB = sb("ssB", [128, NSUB], F32); BssB = Buf("ssB"); rstdB = sb("rstdB", [128, NSUB], F32); BrstdB = Buf("rstdB")
        ssN = {k_: sb("ssN" + k_, [128, NSUB], F32) for k_ in "AB"}; rstdN = {k_: sb("rstdN" + k_, [128, NSUB], F32) for k_ in "AB"}
        BssN = {k_: [Buf("ssN%s%d" % (k_, i)) for i in range(NSUB)] for k_ in "AB"}
        BrstdN = {k_: [Buf("rstdN%s%d" % (k_, i)) for i in range(NSUB)] for k_ in "AB"}
        ssq = sb("ssq", [128, 16], F32); Bssq = Buf("ssq")
        rq = sb("rq", [128, 16], F32); Brq = Buf("rq")
        den = sb("den", [128, 4], F32); Bden = Buf("den")
        rden = sb("rden", [128, 4], F32); Brden = Buf("rden")
        Lg = sb("Lg", [128, NSUB, NE], F32); BLg = Buf("Lg")
        m8 = sb("m8", [128, NSUB, 8], F32); Bm8 = Buf("m8")
        nv1 = sb("nv1", [128, NSUB], F32); Bnv1 = Buf("nv1")
        ex = sb("ex", [128, NSUB, NE], F32); Bex = Buf("ex")
        msk = sb("msk", [128, NSUB, NE], F32); Bmsk = Buf("msk")
        gden = sb("gden", [128, NSUB], F32); Bgden = Buf("gden")
        gates = sb("gates", [128, NSUB, NE], F32); Bgates = Buf("gates")

        psT = st.enter_context(nc.psum_tensor("psT", [128, 2, 1024], BF16)); BpT = [Buf("psT%d" % i) for i in range(2)]
        psF = st.enter_context(nc.psum_tensor("psF", [128, 6, 512], F32)); BpF = [Buf("psF%d" % i) for i in range(6)]
        pst = {"a1": 0, "a2": 0, "b1": 0, "ringA": 0, "cvt": 0, "th": 0, "pt": 0}

        def psA():
            i = pst["a1"]; pst["a1"] = (i + 1) % 4
            return i

        def psA_pair():
            i = pst["a2"]; pst["a2"] = (i + 1) % 2
            return 2 * i

        def psB():
            i = pst["b1"]; pst["b1"] = (i + 1) % 2
            return 4 + i

        T.dma("sp", [(ident[:], ident_d[:, :])], writes=[Bident], sem_on=(Bident, "w"))
        T.dma("sp", [(poolA[:], poolA_d.rearrange("a p t -> p a t"))], writes=[BpoolA], sem_on=(BpoolA, "w"))
        T.dma("sp", [(masks[:].rearrange("p a b -> p (a b)"), masks_d[:, :])], writes=[Bmasks], sem_on=(Bmasks, "w"))
        gsrc = [pool_norm_d, ffn_norm_d[0, :], ffn_norm_d[1, :], pgn_d[0, :], pgn_d[1, :], kv_norm_d, attn_norm_d]
        G_POOL, G_FFN0, G_FFN1, G_PG0, G_PG1, G_KV, G_Q = range(7)
        with nc.allow_non_contiguous_dma(reason="one-time 4KiB gain-vector transposes"):
            T.dma("sp", [(gcol[:, i, :], g.rearrange("(kc p) -> p kc", p=128)) for i, g in enumerate(gsrc)],
                  writes=[Bgcol], sem_on=(Bgcol, "w"))
        T.dma("sp", [(gq_bc[:], q_norm_d.partition_broadcast(128)), (gk_bc[:], k_norm_d.partition_broadcast(128))],
              writes=[Bgqk], sem_on=(Bgqk, "w"))
        T.dma("sp", [(rb_bc[:], router_b_d.partition_broadcast(128))], writes=[Brb], sem_on=(Brb, "w"))
        T.dma("sp", [(sinks_bc[:], sinks_d.partition_broadcast(128))], writes=[Bsinks], sem_on=(Bsinks, "w"))
        T.op("dve", lambda e: e.tensor_reduce(out=mqk[:, 0:1], in_=gq_bc[:], axis=AX.X, op=ALU.max, apply_absolute_value=True),
             reads=[Bgqk], writes=[Bmqk])
        T.op("dve", lambda e: e.tensor_reduce(out=mqk[:, 1:2], in_=gk_bc[:], axis=AX.X, op=ALU.max, apply_absolute_value=True),
             reads=[Bgqk], writes=[Bmqk])
        T.op("dve", lambda e: e.tensor_scalar(out=negc[:], in0=mqk[:, 0:1], scalar1=-8.0, scalar2=mqk[:, 1:2], op0=ALU.mult, op1=ALU.mult),
             reads=[Bmqk], writes=[Bnegc])
        T.op("act", lambda e: e.activation(out=esink[:], in_=sinks_bc[:], func=AF.Exp, bias=negc[:, 0:1]),
             reads=[Bsinks, Bnegc], writes=[Besink])
        T.op("pool", lambda e: e.memset(Vaug[:].rearrange("p a b c -> p (a b c)"), 1.0), writes=BV)

        allr = ringA + ringB
        Ballr = BringA + BringB
        in_slots = [0, 1, 2, 3]
        out_slots = [4, 5, 6, 7]
        pre = {"i": 0}
        cvt_engs = ["act", "dve"]
        STG = 2048

        def cvt(out_ap, in_ap, gain_ap, reads, writes):
            e = cvt_engs[pst["cvt"] % 2]
            pst["cvt"] += 1
            rd = list(reads) + ([Bgcol] if gain_ap is not None else [])
            if e == "act":
                if gain_ap is None:
                    T.op("act", lambda en: en.activation(out=out_ap, in_=in_ap, func=AF.Copy), reads=rd, writes=writes)
                else:
                    T.op("act", lambda en: en.activation(out=out_ap, in_=in_ap, func=AF.Copy, scale=gain_ap), reads=rd, writes=writes)
            else:
                if gain_ap is None:
                    T.op(e, lambda en: en.tensor_copy(out=out_ap, in_=in_ap), reads=rd, writes=writes)
                else:
                    T.op(e, lambda en: en.tensor_scalar(out=out_ap, in0=in_ap, scalar1=gain_ap, scalar2=None, op0=ALU.mult),
                         reads=rd, writes=writes)

        def gc(n, kc):
            return gcol[:, n, kc:kc + 1]

        class Stager:
            def __init__(self, ins_, outs_, q):
                self.ins, self.outs, self.q, self.i = ins_, outs_, q, 0

            def next(self):
                k = self.i; self.i += 1
                fin, Bin = self.ins[k % len(self.ins)]
                fout, Bout = self.outs[k % len(self.outs)]
                return fin, Bin, fout, Bout

        stg1 = Stager([(allr[i][:].bitcast(F32), Ballr[i]) for i in range(4)],
                      [(allr[i][:, 0:STG], Ballr[i]) for i in range(4, 8)], "sp")

        fin0 = allr[0][:].bitcast(F32)
        fin1 = allr[1][:].bitcast(F32)
        T.dma("sp", [(fin0[:, 0:2048].rearrange("p (g c d) -> p g c d", g=4, c=2),
                      pool_w_d.rearrange("g (c p) d -> p g c d", p=128))], writes=[Ballr[0]], sem_on=(Ballr[0], "w"))
        T.dma("sp", [(fin1[:, 0:1024], pool_scale_d.partition_broadcast(128))], writes=[Ballr[1]], sem_on=(Ballr[1], "w"))
        for g in range(4):
            for c in range(2):
                T.op("dve", lambda e, g=g, c=c: e.scalar_tensor_tensor(
                    out=wpool[:, g, c, :], in0=fin0[:, (g * 2 + c) * 256:(g * 2 + c + 1) * 256], scalar=gc(G_POOL, 2 * g + c),
                    in1=fin1[:, g * 256:(g + 1) * 256], op0=ALU.mult, op1=ALU.mult),
                    reads=[Ballr[0], Ballr[1], Bgcol], writes=[Bwpool])
        with nc.allow_non_contiguous_dma(reason="one-time 32KiB router weight load"):
            T.dma("sp", [(wrf[:], router_w_d.rearrange("(kc p) e -> p kc e", p=128))], writes=[Bwrf], sem_on=(Bwrf, "w"))
        T.op("dve", lambda e: e.tensor_tensor(out=wr[:], in0=wrf[:], in1=gcol[:, G_FFN1, :].unsqueeze(2).to_broadcast([128, 8, NE]), op=ALU.mult),
             reads=[Bwrf, Bgcol], writes=[Bwr])

        def pre_rows(stg, W2d, scr, Bscr, nrow_chunks, ncols, gain_idx):
            Wv = W2d.rearrange("(r p) n -> p r n", p=128)
            per = max(1, STG // ncols)
            r0 = 0
            while r0 < nrow_chunks:
                nr = min(per, nrow_chunks - r0)
                fin, Bin, fout, Bout = stg.next()
                yield (stg.q, [], [Bin])
                T.dma(stg.q, [(fin[:, 0:nr * ncols].rearrange("p (r n) -> p r n", r=nr), Wv[:, r0:r0 + nr, :])],
                      writes=[Bin], sem_on=(Bin, "w"))
                if gain_idx is None:
                    cvt(fout[:, 0:nr * ncols], fin[:, 0:nr * ncols], None, [Bin], [Bout])
                else:
                    for rr in range(nr):
                        cvt(fout[:, rr * ncols:(rr + 1) * ncols], fin[:, rr * ncols:(rr + 1) * ncols], gc(gain_idx, r0 + rr), [Bin], [Bout])
                T.dma("pool", [(scr[:, r0:r0 + nr, :], fout[:, 0:nr * ncols].rearrange("p (r n) -> p r n", r=nr))],
                      reads=[Bout], writes=[Bscr], sem_on=(Bout, "r"))
                r0 += nr

        def pre_gu(stg, W2d, nj, scr, Bscr, gain_idx):
            Wv = W2d.rearrange("(kc p) n -> p kc n", p=128)
            for kc in range(8):
                j0 = 0
                while j0 < nj:
                    jn = min(8, nj - j0)
                    w = jn * 128
                    fin, Bin, fout, Bout = stg.next()
                    yield (stg.q, [], [Bin])
                    T.dma(stg.q, [(fin[:, 0:w], Wv[:, kc, j0 * 128:j0 * 128 + w]),
                                  (fin[:, w:2 * w], Wv[:, kc, nj * 128 + j0 * 128:nj * 128 + j0 * 128 + w])],
                          writes=[Bin], sem_on=(Bin, "w"))
                    ov = fout[:, 0:2 * w].rearrange("p (j two c) -> p two j c", two=2, c=128)
                    iv = fin[:, 0:2 * w].rearrange("p (two j c) -> p two j c", two=2, c=128)
                    cvt(ov, iv, gc(gain_idx, kc), [Bin], [Bout])
                    T.dma("pool", [(scr[:, kc, j0 * 256:j0 * 256 + 2 * w], fout[:, 0:2 * w])],
                          reads=[Bout], writes=[Bscr], sem_on=(Bout, "r"))
                    j0 += jn

        def prepass1():
            if last_stage >= STAGES.index("ffn"):
                yield from pre_gu(stg1, w_gu_d, NJ, s_gu, B_s_gu, G_FFN0)
                yield from pre_rows(stg1, w_down_d, s_down, B_s_down, NJ, D, None)
            if last_stage >= STAGES.index("ple0"):
                yield from pre_rows(stg1, pgw_d[0], s_pg[0], B_s_pg[0], 8, D, G_PG0)
                yield from pre_rows(stg1, plew_d[0], s_plew[0], B_s_plew[0], 2, D, None)
            if last_stage >= STAGES.index("attn"):
                yield from pre_rows(stg1, w_kv_d, s_kv, B_s_kv, 8, 512, G_KV)
                yield from pre_rows(stg1, w_q_d, s_q, B_s_q, 8, D, G_Q)
                yield from pre_rows(stg1, w_o_d, s_o, B_s_o, 8, D, None)

        def prepass2(stg):
            if last_stage >= STAGES.index("moe"):
                for e_ in range(NE):
                    yield from pre_gu(stg, we_gu_d[e_], 8, s_egu[e_], B_s_egu[e_], G_FFN1)
                    yield from pre_rows(stg, we_down_d[e_], s_ed[e_], B_s_ed[e_], 8, D, None)
            if last_stage >= STAGES.index("ple1"):
                yield from pre_rows(stg, pgw_d[1], s_pg[1], B_s_pg[1], 8, D, G_PG1)
                yield from pre_rows(stg, plew_d[1], s_plew[1], B_s_plew[1], 2, D, None)

        for _ in prepass1():
            pass

        def P(reads, writes=(), eng="pe"):
            return (eng, list(reads), list(writes))

        def wsA(src_ap, Bsrc):
            i = pst["ringA"]; pst["ringA"] = (i + 1) % NRA
            n = 1
            for d_ in src_ap.shape[1:]:
                n *= d_
            assert n <= SLOT
            dst = ringA[i][:, 0:n]
            if len(src_ap.shape) == 3:
                dst = dst.rearrange("p (a b) -> p a b", a=src_ap.shape[1])
            T.dma("sp", [(dst, src_ap)], reads=[Bsrc], writes=[BringA[i]], sem_on=(BringA[i], "w"))
            return i

        def wsB(i, src_ap, Bsrc):
            n = 1
            for d_ in src_ap.shape[1:]:
                n *= d_
            dst = ringB[i][:, 0:n]
            if len(src_ap.shape) == 3:
                dst = dst.rearrange("p (a b) -> p a b", a=src_ap.shape[1])
            T.dma("sp", [(dst, src_ap)], reads=[Bsrc], writes=[BringB[i]], sem_on=(BringB[i], "w"))

        def norm_stats(hc, Bhc, ss, Bss, rstd, Brstd):
            for s in range(NSUB):
                if s % 2 == 0:
                    T.op("act", lambda e, s=s: e.activation(out=junk[:], in_=hc[:, s, :], func=AF.Square, accum_out=ss[:, s:s + 1]),
                         reads=[Bhc[s]], writes=[Bss], cost=1.3)
                else:
                    T.op("dve", lambda e, s=s: e.scalar_tensor_tensor(out=junk2[:], in0=hc[:, s, :], scalar=1.0, in1=hc[:, s, :], op0=ALU.mult, op1=ALU.mult,
                                                                      accum_out=ss[:, s:s + 1]),
                         reads=[Bhc[s]], writes=[Bss], cost=1.4)
            T.op("dve", lambda e: e.tensor_scalar(out=ss[:], in0=ss[:], scalar1=1.0 / D, scalar2=EPS, op0=ALU.mult, op1=ALU.add),
                 reads=[Bss], writes=[Bss], cost=0.2)
            T.op("act", lambda e: e.activation(out=ss[:], in_=ss[:], func=AF.Sqrt), reads=[Bss], writes=[Bss], cost=0.25, tbl='S')
            T.op("dve", lambda e: e.reciprocal(out=rstd[:], in_=ss[:]), reads=[Bss], writes=[Brstd], cost=0.2)

        BssP = [Buf("ssP%d" % i) for i in range(NSUB)]; BrstdP = [Buf("rstdP%d" % i) for i in range(NSUB)]

        def norm_stats1(hc, Bhc, s, ss, Bss_, rstd, Brstd_):
            sc = ss[:, s:s + 1]
            if s % 2 == 0:
                T.op("act", lambda e: e.activation(out=junk[:], in_=hc[:, s, :], func=AF.Square, accum_out=sc), reads=[Bhc[s]], writes=[Bss_[s]], cost=1.3)
            else:
                T.op("dve", lambda e: e.scalar_tensor_tensor(out=junk2[:], in0=hc[:, s, :], scalar=1.0, in1=hc[:, s, :], op0=ALU.mult, op1=ALU.mult,
                                                             accum_out=sc), reads=[Bhc[s]], writes=[Bss_[s]], cost=1.4)
            T.op("dve", lambda e: e.tensor_scalar(out=sc, in0=sc, scalar1=1.0 / D, scalar2=EPS, op0=ALU.mult, op1=ALU.add),
                 reads=[Bss_[s]], writes=[Bss_[s]], cost=0.2)
            T.op("act", lambda e: e.activation(out=sc, in_=sc, func=AF.Sqrt), reads=[Bss_[s]], writes=[Bss_[s]], cost=0.25, tbl='S')
            T.op("dve", lambda e: e.reciprocal(out=rstd[:, s:s + 1], in_=sc), reads=[Bss_[s]], writes=[Brstd_[s]], cost=0.2)

        def make_u(hc, Bhc, s, ub, rstd, Brstd):
            T.op("dve", lambda e: e.tensor_scalar(out=u[ub][:], in0=hc[:, s, :], scalar1=rstd[:, s:s + 1], scalar2=None, op0=ALU.mult),
                 reads=[Bhc[s], Brstd], writes=[Bu[ub]], cost=1.3)

        def transpose_to(k, src, Bsrc, nch, dst_view, Bdst):
            def f(e):
                for c in range(nch):
                    ins = e.transpose(psT[:, k, c * 128:(c + 1) * 128], src[:, c * 128:(c + 1) * 128], ident[:])
                return ins
            T.op("pe", f, reads=[Bsrc, Bident], writes=[BpT[k]], cost=0.08 * nch)
            pv = psT[:, k, 0:nch * 128].rearrange("p (a b) -> p a b", a=nch)
            T.op("act", lambda e: e.activation(out=dst_view, in_=pv, func=AF.Copy), reads=[BpT[k]], writes=[Bdst], cost=0.3 + 0.09 * nch)

        LBL = [""]

        def norm_sub(hc, Bhc, s, stream, ub0_=None):
            T.tag = stream + ":norm"
            ub0, uT_, BuT_, k = (0, uTa, BuTa, 0) if stream == "A" else (3, uTb, BuTb, 1)
            if ub0_ is not None:
                ub0 = ub0_
            ss, rstd, Bs, Br = ssN[stream], rstdN[stream], BssN[stream][s], BrstdN[stream][s]
            sc = ss[:, s:s + 1]
            if s % 2 == 0:
                T.op("act", lambda e: e.activation(out=junk[:], in_=hc[:, s, :], func=AF.Square, accum_out=sc), reads=[Bhc[s]], writes=[Bs], cost=1.3)
            else:
                T.op("dve", lambda e: e.scalar_tensor_tensor(out=junk2[:], in0=hc[:, s, :], scalar=1.0, in1=hc[:, s, :], op0=ALU.mult, op1=ALU.mult,
                                                             accum_out=sc), reads=[Bhc[s]], writes=[Bs], cost=1.4)
            T.op("dve", lambda e: e.tensor_scalar(out=sc, in0=sc, scalar1=1.0 / D, scalar2=EPS, op0=ALU.mult, op1=ALU.add), reads=[Bs], writes=[Bs], cost=0.2)
            T.op("act", lambda e: e.activation(out=sc, in_=sc, func=AF.Sqrt), reads=[Bs], writes=[Bs], cost=0.25, tbl='S')
            T.op("dve", lambda e: e.reciprocal(out=rstd[:, s:s + 1], in_=sc), reads=[Bs], writes=[Br], cost=0.2)
            ub = ub0 + s % 2
            T.op("dve", lambda e: e.tensor_scalar(out=u[ub][:], in0=hc[:, s, :], scalar1=rstd[:, s:s + 1], scalar2=None, op0=ALU.mult),
                 reads=[Bhc[s], Br], writes=[Bu[ub]], cost=1.3)

        def norm_T(s, stream, ub0_=None):
            T.tag = stream + ":norm:" + LBL[0]
            ub0, uT_, BuT_, k = (0, uTa, BuTa, 0) if stream == "A" else (3, uTb, BuTb, 1)
            if ub0_ is not None:
                ub0 = ub0_
            ub = ub0 + s % 2
            yield P([Bu[ub]], [BpT[k]])
            transpose_to(k, u[ub], Bu[ub], 8, uT_[:, :, s * 128:(s + 1) * 128], BuT_[s])

        def norm_to_uT(hc, Bhc, stream, label="first"):
            LBL[0] = label
            for s in range(NSUB):
                norm_sub(hc, Bhc, s, stream)
                yield from norm_T(s, stream)

        class LagNorm:
            def __init__(self, hc, Bhc, stream="A", label="lag", ub0=None):
                self.hc, self.Bhc, self.stream, self.pending, self.label, self.ub0 = hc, Bhc, stream, [], label, ub0

            def done(self, s, keep=1):
                norm_sub(self.hc, self.Bhc, s, self.stream, self.ub0)
                self.pending.append(s)
                yield from self.flush(keep)

            def flush(self, keep=0):
                LBL[0] = self.label
                while len(self.pending) > keep:
                    yield from norm_T(self.pending.pop(0), self.stream, self.ub0)

            def need(self, s):
                LBL[0] = self.label
                while self.pending and self.pending[0] <= s:
                    yield from norm_T(self.pending.pop(0), self.stream, self.ub0)

        def up_chunk(si, nch, jj, j, hb):
            sv = ringA[si][:, 0:8 * nch * 256].rearrange("p (k j c) -> p k j c", k=8, j=nch)
            ba = psA(); bg = psA()
            tb = pst["th"] % 2; pst["th"] += 1
            yield P([BringA[si]] + BuTa, [BpF[ba], BpF[bg]])
            T.tag = "A:up"

            def fa(e):
                for kc in range(8):
                    ins = e.matmul(psF[:, ba, :], lhsT=sv[:, kc, jj, 0:128], rhs=uTa[:, kc, :], start=(kc == 0), stop=(kc == 7))
                return ins

            def fg(e):
                for kc in range(8):
                    ins = e.matmul(psF[:, bg, :], lhsT=sv[:, kc, jj, 128:256], rhs=uTa[:, kc, :], start=(kc == 0), stop=(kc == 7))
                return ins
            T.op("pe", fa, reads=[BringA[si]] + BuTa, writes=[BpF[ba]], cost=1.75)
            T.op("pe", fg, reads=[BringA[si]] + BuTa, writes=[BpF[bg]], cost=1.75)
            T.op("act", lambda e: e.activation(out=th[tb][:], in_=psF[:, ba, :], func=AF.Tanh, scale=0.5), reads=[BpF[ba]], writes=[Bth[tb]], cost=0.65, tbl='E')
            T.op("dve", lambda e: e.scalar_tensor_tensor(out=mm[tb][:], in0=th[tb][:], scalar=1.0, in1=psF[:, bg, :], op0=ALU.add, op1=ALU.mult),
                 reads=[Bth[tb], BpF[bg]], writes=[Bmm[tb]])
            T.op("dve", lambda e: e.scalar_tensor_tensor(out=hm[hb][:, j, :], in0=psF[:, ba, :], scalar=0.5, in1=mm[tb][:], op0=ALU.mult, op1=ALU.mult),
                 reads=[BpF[ba], Bmm[tb]], writes=[Bhm[hb]])

        def down_units(hc, Bhc, down_src, Bdown_src, j0, nj, gate_e, hb, hook=None):
            pieces = []
            jx = 0
            while jx < nj:
                n = min(4, nj - jx)
                pieces.append((wsA(down_src[:, j0 + jx:j0 + jx + n, :], Bdown_src), n))
                jx += n
            for s in range(NSUB):
                for half in range(2):
                    b = psA()
                    yield P([Bhm[hb]] + [BringA[si] for si, _ in pieces], [BpF[b]])
                    T.tag = "A:down"

                    def fd(e, s=s, half=half, b=b):
                        jx = 0
                        for si, n in pieces:
                            dv = ringA[si][:, 0:n * D].rearrange("p (j n) -> p j n", j=n)
                            for q in range(n):
                                ins = e.matmul(psF[:, b, :], lhsT=hm[hb][:, jx, s * 128:(s + 1) * 128], rhs=dv[:, q, half * 512:(half + 1) * 512],
                                               start=(jx == 0), stop=(jx == nj - 1))
                                jx += 1
                        return ins
                    T.op("pe", fd, reads=[Bhm[hb]] + [BringA[si] for si, _ in pieces], writes=[BpF[b]], cost=0.216 * nj)
                    hv = hc[:, s, half * 512:(half + 1) * 512]
                    if gate_e is None:
                        T.op("dve", lambda e, b=b, hv=hv: e.tensor_tensor(out=hv, in0=hv, in1=psF[:, b, :], op=ALU.add),
                             reads=[BpF[b], Bhc[s]], writes=[Bhc[s]])
                    else:
                        T.op("dve", lambda e, b=b, hv=hv, s=s: e.scalar_tensor_tensor(out=hv, in0=psF[:, b, :], scalar=gates[:, s, gate_e:gate_e + 1],
                                                                                      in1=hv, op0=ALU.mult, op1=ALU.add),
                             reads=[BpF[b], Bhc[s], Bgates], writes=[Bhc[s]])
                if hook is not None:
                    yield from hook(s)

        def ffn_groups(hc, Bhc, groups, tail_hook=None, extra=None):
            pending = None
            for gi, (up_fn, Bup, down_src, Bdown, j0, nj, gate_e) in enumerate(groups):
                hb = gi % 2
                j = 0
                while j < nj:
                    nch = min(2, nj - j)
                    si = wsA(up_fn(j0 + j, nch), Bup)
                    for jj in range(nch):
                        yield from up_chunk(si, nch, jj, j, hb)
                        j += 1
                        if pending is not None:
                            yield from pending
                            pending = None
                pending = down_units(hc, Bhc, down_src, Bdown, j0, nj, gate_e, hb,
                                     hook=(tail_hook if gi == len(groups) - 1 else None))
                if gi == 0 and extra is not None:
                    yield from extra
            yield from pending

        def issue_p(l, t0):
            T.dma("pool", [(pb[:].rearrange("p (s f) -> p s f", s=NSUB), p_d[l][t0:t0 + TM, :].rearrange("(s p) f -> p s f", p=128))],
                  writes=[Bpb], sem_on=(Bpb, "w"))

        def load_p(l, t0, issued=False):
            if not issued:
                issue_p(l, t0)
            for s in range(NSUB):
                yield P([Bpb], [BpT[0]])
                T.tag = "A:pT"
                transpose_to(0, pb[:, s * 256:(s + 1) * 256], Bpb, 2, pT[:, :, s * 128:(s + 1) * 128], BpT_)

        def ple(hc, Bhc, l, t0, lag=None, tail_hook=None, p_loaded=False):
            if lag is None:
                yield from norm_to_uT(hc, Bhc, "A")
            if not p_loaded:
                yield from load_p(l, t0)
            iw = wsA(s_plew[l], B_s_plew[l])
            wv = ringA[iw][:, 0:2 * D].rearrange("p (k n) -> p k n", k=2)
            sis = [wsA(s_pg[l][:, :, half * 512:(half + 1) * 512], B_s_pg[l]) for half in range(2)]
            for s in range(NSUB):
                if lag is not None:
                    yield from lag.need(s)
                for half in range(2):
                    si = sis[half]
                    sv = ringA[si][:, 0:8 * 512].rearrange("p (k n) -> p k n", k=8)
                    bgt = psA(); be = psA()
                    tb = pst["th"] % 2; pst["th"] += 1
                    yield P([BuTa[s], BringA[si], BpT_, BringA[iw]], [BpF[bgt], BpF[be]])
                    T.tag = "A:ple"

                    def fgate(e, s=s, bgt=bgt, sv=sv):
                        for kc in range(8):
                            ins = e.matmul(psF[:, bgt, :], lhsT=uTa[:, kc, s * 128:(s + 1) * 128], rhs=sv[:, kc, :], start=(kc == 0), stop=(kc == 7))
                        return ins

                    def fe(e, s=s, half=half, be=be):
                        for kc in range(2):
                            ins = e.matmul(psF[:, be, :], lhsT=pT[:, kc, s * 128:(s + 1) * 128], rhs=wv[:, kc, half * 512:(half + 1) * 512],
                                           start=(kc == 0), stop=(kc == 1))
                        return ins
                    T.op("pe", fgate, reads=[BuTa[s], BringA[si]], writes=[BpF[bgt]], cost=1.75)
                    T.op("pe", fe, reads=[BpT_, BringA[iw]], writes=[BpF[be]], cost=0.45)
                    T.op("act", lambda e, bgt=bgt, tb=tb: e.activation(out=th[tb][:], in_=psF[:, bgt, :], func=AF.Tanh, scale=0.5),
                         reads=[BpF[bgt]], writes=[Bth[tb]], cost=0.65, tbl='E')
                    T.op("dve", lambda e, be=be, tb=tb: e.scalar_tensor_tensor(out=mm[tb][:], in0=th[tb][:], scalar=1.0, in1=psF[:, be, :],
                                                                               op0=ALU.add, op1=ALU.mult),
                         reads=[Bth[tb], BpF[be]], writes=[Bmm[tb]])
                    hv = hc[:, s, half * 512:(half + 1) * 512]
                    T.op("dve", lambda e, tb=tb, hv=hv: e.scalar_tensor_tensor(out=hv, in0=mm[tb][:], scalar=0.5, in1=hv, op0=ALU.mult, op1=ALU.add),
                         reads=[Bmm[tb], Bhc[s]], writes=[Bhc[s]])
                    if half == 1 and tail_hook is not None:
                        yield from tail_hook(s)

        def qk_norm_rope(xf, Bxf, H, tab0, s, out_bf, Bout):
            n = H * 64
            xv = xf[:, 0:n].rearrange("p (h d) -> p h d", h=H)
            T.op("act", lambda e: e.activation(out=rtt[:, 0:n], in_=xf[:, 0:n], func=AF.Square), reads=[Bxf], writes=Brt)
            T.op("dve", lambda e: e.tensor_reduce(out=ssq[:, 0:H], in_=rtt[:, 0:n].rearrange("p (h d) -> p h d", h=H), axis=AX.X, op=ALU.add),
                 reads=Brt, writes=[Bssq])
            T.op("dve", lambda e: e.tensor_scalar(out=ssq[:, 0:H], in0=ssq[:, 0:H], scalar1=1.0 / 64, scalar2=EPS, op0=ALU.mult, op1=ALU.add),
                 reads=[Bssq], writes=[Bssq], cost=0.2)
            T.op("act", lambda e: e.activation(out=ssq[:, 0:H], in_=ssq[:, 0:H], func=AF.Sqrt), reads=[Bssq], writes=[Bssq], cost=0.25, tbl='S')
            T.op("dve", lambda e: e.reciprocal(out=rq[:, 0:H], in_=ssq[:, 0:H]), reads=[Bssq], writes=[Brq], cost=0.25)
            T.op("dve", lambda e: e.tensor_tensor(out=xv, in0=xv, in1=rq[:, 0:H].unsqueeze(2).to_broadcast([128, H, 64]), op=ALU.mult),
                 reads=[Bxf, Brq], writes=[Bxf])
            x1 = xv[:, :, 0:32]; x2 = xv[:, :, 32:64]
            tb = lambda i: rtab[:, tab0 + i, s, :].unsqueeze(1).to_broadcast([128, H, 32])
            t1 = rtt[:, 0:H * 32].rearrange("p (h d) -> p h d", h=H)
            t2 = rtt[:, 512:512 + H * 32].rearrange("p (h d) -> p h d", h=H)
            ov = out_bf[:, 0:n].rearrange("p (h d) -> p h d", h=H)
            T.op("dve", lambda e: e.tensor_tensor(out=t1, in0=x1, in1=tb(0), op=ALU.mult), reads=[Bxf, Brtab], writes=[Brt[0]])
            T.op("pool", lambda e: e.tensor_tensor(out=t2, in0=x2, in1=tb(1), op=ALU.mult), reads=[Bxf, Brtab], writes=[Brt[1]])
            T.op("dve", lambda e: e.tensor_tensor(out=ov[:, :, 0:32], in0=t1, in1=t2, op=ALU.subtract), reads=Brt, writes=[Bout])
            T.op("dve", lambda e: e.tensor_tensor(out=t1, in0=x2, in1=tb(2), op=ALU.mult), reads=[Bxf, Brtab], writes=[Brt[0]])
            T.op("pool", lambda e: e.tensor_tensor(out=t2, in0=x1, in1=tb(3), op=ALU.mult), reads=[Bxf, Brtab], writes=[Brt[1]])
            T.op("dve", lambda e: e.tensor_tensor(out=ov[:, :, 32:64], in0=t1, in1=t2, op=ALU.add), reads=Brt, writes=[Bout])

        def att(m, hc, Bhc):
            t0 = m * TM
            wsB(0, s_kv, B_s_kv)
            wsB(1, s_q[:, :, 0:512], B_s_q)
            wsB(2, s_q[:, :, 512:1024], B_s_q)
            T.dma("sp", [(cs[:], cos_d[t0:t0 + TM, :].rearrange("(s p) f -> p s f", p=128))], writes=[Bcs], sem_on=(Bcs, "w"))
            T.dma("sp", [(sn[:], sin_d[t0:t0 + TM, :].rearrange("(s p) f -> p s f", p=128))], writes=[Bsn], sem_on=(Bsn, "w"))
            for qi, gbc in enumerate((gq_bc, gk_bc)):
                for ti, (tab, lo) in enumerate(((cs, 0), (sn, 32), (cs, 32), (sn, 0))):
                    T.op("pool", lambda e, qi=qi, ti=ti, tab=tab, lo=lo, gbc=gbc: e.tensor_tensor(
                        out=rtab[:, qi * 4 + ti, :, :], in0=tab[:], in1=gbc[:, lo:lo + 32].unsqueeze(1).to_broadcast([128, NSUB, 32]), op=ALU.mult),
                        reads=[Bcs, Bsn, Bgqk], writes=[Brtab])
            yield from norm_to_uT(hc, Bhc, "B")
            def q_transposes(sq):
                yield P([Bqr], [BpT[1]])
                T.tag = "B:qT"
                transpose_to(1, qr, Bqr, 8, QT[:, :, sq * 128:(sq + 1) * 128], BQT)

            kvv = ringB[0][:, 0:8 * 512].rearrange("p (k n) -> p k n", k=8)
            qv = [ringB[1 + h_][:, 0:8 * 512].rearrange("p (k n) -> p k n", k=8) for h_ in range(2)]
            for s in range(NSUB):
                bkv = psB()
                yield P([BuTb[s], BringB[0]], [BpF[bkv]])
                T.tag = "B:kvproj"

                def fkv(e, s=s, bkv=bkv):
                    for kc in range(8):
                        ins = e.matmul(psF[:, bkv, :], lhsT=uTb[:, kc, s * 128:(s + 1) * 128], rhs=kvv[:, kc, :], start=(kc == 0), stop=(kc == 7))
                    return ins
                T.op("pe", fkv, reads=[BuTb[s], BringB[0]], writes=[BpF[bkv]], cost=1.75)
                T.op("act", lambda e, bkv=bkv: e.activation(out=kf[:], in_=psF[:, bkv, 0:256], func=AF.Copy), reads=[BpF[bkv]], writes=[Bkf])
                T.op("act", lambda e, bkv=bkv, s=s: e.activation(out=Vaug[:, 1 + s, :, 0:64], in_=psF[:, bkv, 256:512].rearrange("p (h d) -> p h d", h=4),
                                                                 func=AF.Copy), reads=[BpF[bkv]], writes=[BV[1 + s]])
                if s >= 1:
                    yield from q_transposes(s - 1)
                for half in range(2):
                    bq = psB()
                    yield P([BuTb[s], BringB[1 + half]], [BpF[bq]])
                    T.tag = "B:qproj"

                    def fq(e, s=s, half=half, bq=bq):
                        for kc in range(8):
                            ins = e.matmul(psF[:, bq, :], lhsT=uTb[:, kc, s * 128:(s + 1) * 128], rhs=qv[half][:, kc, :], start=(kc == 0), stop=(kc == 7))
                        return ins
                    T.op("pe", fq, reads=[BuTb[s], BringB[1 + half]], writes=[BpF[bq]], cost=1.75)
                    T.op("act", lambda e, half=half, bq=bq: e.activation(out=qf[:, half * 512:(half + 1) * 512], in_=psF[:, bq, :], func=AF.Copy),
                         reads=[BpF[bq]], writes=[Bqf])
                qk_norm_rope(kf, Bkf, 4, 4, s, kr, Bkr)
                yield P([Bkr], [BpT[1]])
                T.tag = "B:kT"
                transpose_to(1, kr, Bkr, 2, KT[:, :, (1 + s) * 128:(2 + s) * 128], BK[1 + s])
                qk_norm_rope(qf, Bqf, 16, 0, s, qr, Bqr)
            yield from q_transposes(NSUB - 1)
            wsB(1, s_o[:, :, 0:512], B_s_o)
            wsB(2, s_o[:, :, 512:1024], B_s_o)
            ovw = [ringB[1 + h_][:, 0:8 * 512].rearrange("p (k n) -> p k n", k=8) for h_ in range(2)]
            for b in range(NSUB):
                first = (m == 0 and b == 0)
                kbs = [1 + b] if first else [b, 1 + b]
                n = len(kbs)
                for hk in range(4):
                    r0 = 0 if hk % 2 == 0 else 64
                    kch = hk // 2
                    qc0 = 4 * (hk // 2)
                    pos0 = 8 * (hk // 2) + (hk % 2)
                    pb_ = pst["pt"] % 2; pst["pt"] += 1
                    for i, kb in enumerate(kbs):
                        ps = psB()
                        yield P([BK[kb], BQT], [BpF[ps], BPT[pb_]])
                        T.tag = "B:S"
                        T.op("pe", lambda e, ps=ps, kb=kb: e.matmul(psF[:, ps, :].rearrange("p (a q) -> p a q", a=4),
                                                                   lhsT=KT[r0:r0 + 64, kch, kb * 128:(kb + 1) * 128],
                                                                   rhs=QT[r0:r0 + 64, qc0:qc0 + 4, b * 128:(b + 1) * 128], start=True, stop=True),
                             reads=[BK[kb], BQT], writes=[BpF[ps]], cost=0.3)
                        T.op("act", lambda e, ps=ps, i=i: e.activation(out=PT[pb_][:, i * 512:(i + 1) * 512], in_=psF[:, ps, :], func=AF.Exp,
                                                                      bias=negc[:, 0:1], scale=0.125),
                             reads=[BpF[ps], Bnegc], writes=[BPT[pb_]], cost=0.65, tbl='E')
                    pv4 = PT[pb_][:, 0:n * 512].rearrange("p (a g q) -> p a g q", a=n, g=4)
                    mk = masks[:, 2 - n:2, :].unsqueeze(2).to_broadcast([128, n, 4, 128])
                    T.op("dve", lambda e, pv4=pv4, mk=mk: e.tensor_tensor(out=pv4, in0=pv4, in1=mk, op=ALU.mult), reads=[BPT[pb_], Bmasks], writes=[BPT[pb_]],
                         cost=0.7)
                    po = psB()
                    yield P([BPT[pb_]] + [BV[kb] for kb in kbs], [BpF[po]])
                    T.tag = "B:PV"

                    def fo(e, po=po, pb_=pb_, kbs=kbs, hk=hk, n=n):
                        for g in range(4):
                            for i, kb in enumerate(kbs):
                                ins = e.matmul(psF[:, po, g * 66:g * 66 + 65], lhsT=PT[pb_][:, i * 512 + g * 128:i * 512 + (g + 1) * 128],
                                               rhs=Vaug[:, kb, hk, 0:65], start=(i == 0), stop=(i == n - 1))
                        return ins
                    T.op("pe", fo, reads=[BPT[pb_]] + [BV[kb] for kb in kbs], writes=[BpF[po]], cost=0.07 * 4 * n)
                    pov = psF[:, po, 0:264].rearrange("p (g d) -> p g d", g=4)
                    T.op("dve", lambda e, pov=pov, pos0=pos0: e.tensor_tensor(out=den[:], in0=pov[:, :, 64], in1=esink[:, pos0:pos0 + 7:2], op=ALU.add),
                         reads=[BpF[po], Besink], writes=[Bden], cost=0.2)
                    T.op("dve", lambda e: e.reciprocal(out=rden[:], in_=den[:]), reads=[Bden], writes=[Brden], cost=0.2)
                    o16 = Ot[:].rearrange("p (h d) -> p h d", h=16)
                    T.op("dve", lambda e, pov=pov, pos0=pos0, o16=o16: e.tensor_tensor(out=o16[:, pos0:pos0 + 7:2, :], in0=pov[:, :, 0:64],
                                                                                      in1=rden[:].unsqueeze(2).to_broadcast([128, 4, 64]), op=ALU.mult),
                         reads=[BpF[po], Brden], writes=[BO], cost=0.4)
                yield P([BO], [BpT[1]])
                T.tag = "B:OT"
                transpose_to(1, Ot, BO, 8, OT[:], BOT)
                for half in range(2):
                    b2 = psB()
                    yield P([BOT, BringB[1 + half]], [BpF[b2]])
                    T.tag = "B:oproj"

                    def fop(e, half=half, b2=b2):
                        for kc in range(8):
                            ins = e.matmul(psF[:, b2, :], lhsT=OT[:, kc, :], rhs=ovw[half][:, kc, :], start=(kc == 0), stop=(kc == 7))
                        return ins
                    T.op("pe", fop, reads=[BOT, BringB[1 + half]], writes=[BpF[b2]], cost=1.75)
                    hv = hc[:, b, half * 512:(half + 1) * 512]
                    T.op("dve", lambda e, b2=b2, hv=hv: e.tensor_tensor(out=hv, in0=hv, in1=psF[:, b2, :], op=ALU.add), reads=[BpF[b2], Bhc[b]], writes=[Bhc[b]])
            T.op("pool", lambda e: e.tensor_copy(out=KT[:, :, 0:128], in_=KT[:, :, 512:640]), reads=[BK[4]], writes=[BK[0]])
            T.op("pool", lambda e: e.tensor_copy(out=Vaug[:, 0, :, :], in_=Vaug[:, 4, :, :]), reads=[BV[4]], writes=[BV[0]])

        def moe(m, hc, Bhc, next_x=None):
            t0 = m * TM
            do_ple1_pre = last_stage >= STAGES.index("ple1") and last_stage >= STAGES.index("moe")
            if last_stage >= STAGES.index("moe"):
                yield from norm_to_uT(hc, Bhc, "A")
                bl = psA()
                yield P(BuTa, [BpF[bl]])
                T.tag = "A:router"

                def fl(e):
                    for s in range(NSUB):
                        for kc in range(8):
                            ins = e.matmul(psF[:, bl, s * 8:(s + 1) * 8], lhsT=uTa[:, kc, s * 128:(s + 1) * 128], rhs=wr[:, kc, :], start=(kc == 0), stop=(kc == 7))
                    return ins
                T.op("pe", fl, reads=BuTa + [Bwr], writes=[BpF[bl]], cost=2.0)
                T.op("dve", lambda e: e.tensor_tensor(out=Lg[:], in0=psF[:, bl, 0:32].rearrange("p (s e) -> p s e", s=NSUB),
                                                      in1=rb_bc[:].unsqueeze(1).to_broadcast([128, NSUB, NE]), op=ALU.add), reads=[BpF[bl], Brb], writes=[BLg])
                for s in range(NSUB):
                    T.op("dve", lambda e, s=s: e.max(out=m8[:, s, :], in_=Lg[:, s, :]), reads=[BLg], writes=[Bm8])
                T.op("dve", lambda e: e.tensor_scalar(out=nv1[:], in0=m8[:, :, 0], scalar1=-1.0, scalar2=None, op0=ALU.mult), reads=[Bm8], writes=[Bnv1])
                for s in range(NSUB):
                    T.op("act", lambda e, s=s: e.activation(out=ex[:, s, :], in_=Lg[:, s, :], func=AF.Exp, bias=nv1[:, s:s + 1]), reads=[BLg, Bnv1], writes=[Bex], cost=0.25, tbl='E')
                    T.op("dve", lambda e, s=s: e.tensor_scalar(out=msk[:, s, :], in0=Lg[:, s, :], scalar1=m8[:, s, 1:2], scalar2=None, op0=ALU.is_ge),
                         reads=[BLg, Bm8], writes=[Bmsk])
                T.op("dve", lambda e: e.tensor_tensor(out=ex[:], in0=ex[:], in1=msk[:], op=ALU.mult), reads=[Bex, Bmsk], writes=[Bex])
                T.op("dve", lambda e: e.tensor_reduce(out=gden[:], in_=ex[:], axis=AX.X, op=ALU.add), reads=[Bex], writes=[Bgden])
                T.op("dve", lambda e: e.reciprocal(out=gden[:], in_=gden[:]), reads=[Bgden], writes=[Bgden])
                T.op("dve", lambda e: e.tensor_tensor(out=gates[:], in0=ex[:], in1=gden[:].unsqueeze(2).to_broadcast([128, NSUB, NE]), op=ALU.mult),
                     reads=[Bex, Bgden], writes=[Bgates])
                if do_ple1_pre:
                    issue_p(1, t0)
                    pl = load_p(1, t0, issued=True)
                groups = [((lambda j, n, e_=e_: s_egu[e_][:, :, j * 256:(j + n) * 256]), B_s_egu[e_], s_ed[e_], B_s_ed[e_], 0, 8, e_) for e_ in range(NE)]
                do_ple1 = last_stage >= STAGES.index("ple1")
                ln = LagNorm(hc, Bhc, label="ple1")
                yield from ffn_groups(hc, Bhc, groups, tail_hook=ln.done if do_ple1 else None, extra=(pl if do_ple1_pre else None))

            ln = None if last_stage < STAGES.index("moe") else ln

            def finish(s):
                T.dma("pool", [(out_d[t0 + s * 128:t0 + (s + 1) * 128, :], hc[:, s, :])], reads=[Bhc[s]], sem_on=(Bhc[s], "r"))
                if next_x is not None:
                    t1 = next_x * TM
                    T.dma("sp", [(hc[:, s, :], x_d[t1 + s * 128:t1 + (s + 1) * 128, :])], writes=[Bhc[s]], sem_on=(Bhc[s], "w"))
                if False:
                    yield
            if last_stage >= STAGES.index("ple1"):
                yield from ple(hc, Bhc, 1, t0, lag=(ln if last_stage >= STAGES.index("moe") else None), tail_hook=finish, p_loaded=do_ple1_pre)
            else:
                for s in range(NSUB):
                    yield from finish(s)

        def layer0(m, hc, Bhc, x_loaded=False):
            t0 = m * TM
            do_ffn = last_stage >= STAGES.index("ffn")
            do_ple0 = last_stage >= STAGES.index("ple0")
            if not x_loaded:
                for s in range(NSUB):
                    T.dma("sp", [(hc[:, s, :], x_d[t0 + s * 128:t0 + (s + 1) * 128, :])], writes=[Bhc[s]], sem_on=(Bhc[s], "w"))
            lnp = LagNorm(hc, Bhc, label="ffn", ub0=5)
            if last_stage >= STAGES.index("pool"):
                norm_stats1(hc, Bhc, 0, ssA, BssP, rstdA, BrstdP)
                make_u(hc, Bhc, 0, 0, rstdA, BrstdP[0])
                norm_stats1(hc, Bhc, 1, ssA, BssP, rstdA, BrstdP)
                for s in range(NSUB):
                    cur = s % 2
                    prev = 2 if s == 0 else (s - 1) % 2
                    first = (m == 0 and s == 0)
                    pd = psA_pair()
                    pdv = psF[:, pd:pd + 2, :].rearrange("p a (c t) -> p (a c) t", t=128)
                    yield P([Bu[cur]] + ([] if first else [Bu[prev]]), [BpF[pd], BpF[pd + 1]])
                    T.tag = "A:pool:d"

                    def fpool(e, cur=cur, prev=prev, first=first, pdv=pdv):
                        for c in range(8):
                            g = c // 2
                            if first:
                                ins = e.matmul(pdv[:, c, :], lhsT=u[cur][:, c * 128:(c + 1) * 128], rhs=poolA[:, 8 + g, :], start=True, stop=True)
                            else:
                                e.matmul(pdv[:, c, :], lhsT=u[cur][:, c * 128:(c + 1) * 128], rhs=poolA[:, g, :], start=True, stop=False)
                                ins = e.matmul(pdv[:, c, :], lhsT=u[prev][:, c * 128:(c + 1) * 128], rhs=poolA[:, 4 + g, :], start=False, stop=True)
                        return ins
                    T.op("pe", fpool, reads=[Bu[cur], BpoolA] + ([] if first else [Bu[prev]]), writes=[BpF[pd], BpF[pd + 1]], cost=1.4)
                    if s + 1 < NSUB:
                        make_u(hc, Bhc, s + 1, (s + 1) % 2, rstdA, BrstdP[s + 1])
                    if s + 2 < NSUB:
                        norm_stats1(hc, Bhc, s + 2, ssA, BssP, rstdA, BrstdP)
                    T.op("act", lambda e, pdv=pdv: e.activation(out=dT[:], in_=pdv, func=AF.Copy), reads=[BpF[pd], BpF[pd + 1]], writes=[BdT], cost=1.2)
                    py = psA_pair()
                    yield P([BdT], [BpF[py], BpF[py + 1]])
                    T.tag = "A:pool:y"

                    def fy(e, py=py):
                        for g in range(4):
                            for cc in range(2):
                                ins = e.matmul(psF[:, py + g // 2, (g % 2) * 256:(g % 2 + 1) * 256], lhsT=dT[:, 2 * g + cc, :], rhs=wpool[:, g, cc, :],
                                               start=(cc == 0), stop=(cc == 1))
                        return ins
                    T.op("pe", fy, reads=[BdT, Bwpool], writes=[BpF[py], BpF[py + 1]], cost=1.2)
                    T.op("dve", lambda e, s=s, py=py: e.tensor_tensor(out=hc[:, s, :].rearrange("p (a n) -> p a n", a=2), in0=hc[:, s, :].rearrange("p (a n) -> p a n", a=2),
                                                                      in1=psF[:, py:py + 2, :], op=ALU.add),
                         reads=[BpF[py], BpF[py + 1], Bhc[s]], writes=[Bhc[s]], cost=1.4)
                    if do_ffn and s >= 1:
                        yield from lnp.done(s - 1, keep=1)
                T.op("pool", lambda e: e.tensor_copy(out=u[2][:], in_=u[1][:]), reads=[Bu[1]], writes=[Bu[2]])
                if do_ffn:
                    yield from lnp.done(NSUB - 1, keep=0)
            if do_ffn:
                if last_stage < STAGES.index("pool"):
                    yield from norm_to_uT(hc, Bhc, "A")
                if do_ple0:
                    issue_p(0, t0)
                    pl = load_p(0, t0, issued=True)
                groups = [((lambda j, n: s_gu[:, :, j * 256:(j + n) * 256]), B_s_gu, s_down, B_s_down, j0, nj, None) for (j0, nj) in ((0, 8), (8, 8), (16, 6))]
                ln0 = LagNorm(hc, Bhc, label="ple0")
                yield from ffn_groups(hc, Bhc, groups, tail_hook=ln0.done if do_ple0 else None, extra=(pl if do_ple0 else None))
            if do_ple0:
                yield from ple(hc, Bhc, 0, t0, lag=(ln0 if do_ffn else None), p_loaded=do_ffn)
            if False:
                yield

        def run(gen):
            for _ in gen:
                pass

        def chain(*gens):
            for g_ in gens:
                yield from g_

        def merge(gb, ga):
            def nxt(g_):
                try:
                    return next(g_)
                except StopIteration:
                    return None

            def est(p):
                eng, rd, wr = p
                return max(T.clock[eng], T.ready_time(rd, wr) + 0.1)
            pa = nxt(ga); pb = nxt(gb)
            while pa is not None or pb is not None:
                if pb is None:
                    pa = nxt(ga)
                elif pa is None:
                    pb = nxt(gb)
                elif est(pb) <= est(pa) + BIAS:
                    pb = nxt(gb)
                else:
                    pa = nxt(ga)

        ctx = lambda m: (hT[m % 2], Bh[m % 2])
        do_att = last_stage >= STAGES.index("attn")
        if not do_att:
            run(prepass2(stg1))
            for m in range(n_macro):
                run(layer0(m, *ctx(m)))
                run(moe(m, *ctx(m)))
        else:
            Bo = [Buf("c_out%d" % i) for i in range(4)]
            uTb_flat = uTb[:].rearrange("p a b -> p (a b)")
            stg2 = Stager([(ringB[0][:].bitcast(F32), BringB[0]), (ringB[1][:].bitcast(F32), BringB[1]),
                           (QT[:].rearrange("p a b -> p (a b)").bitcast(F32), BQT)],
                          [(ringB[2][:, 0:STG], Bo[0]), (ringB[2][:, STG:2 * STG], Bo[1]), (uTb_flat[:, 0:STG], Bo[2]), (uTb_flat[:, STG:2 * STG], Bo[3])],
                          "sp")
            for b_ in Bo[0:2]:
                T.absorb(b_, [BringB[2]])
            merge(prepass2(stg2), layer0(0, *ctx(0)))
            T.absorb(BringB[2], Bo[0:2])
            for b_ in BuTb:
                T.absorb(b_, Bo[2:4])
            for m in range(n_macro):
                a_parts = []
                if m >= 1:
                    a_parts.append(moe(m - 1, *ctx(m - 1), next_x=(m + 1 if m + 1 < n_macro else None)))
                if m + 1 < n_macro:
                    a_parts.append(layer0(m + 1, *ctx(m + 1), x_loaded=(m >= 1)))
                merge(att(m, *ctx(m)), chain(*a_parts))
                SIMLOG.append((m, dict(T.clock), dict(T.gaps)))
            run(moe(n_macro - 1, *ctx(n_macro - 1)))
        T.wait_all("pool", Bh[0] + Bh[1])
        build.stats = (T.ninst, T.nwait, T.nsem, nc.sbuf_bytes_remaining)
        build.sim = dict(T.clock)
        build.busy = dict(T.busy)
        build.gaps = dict(T.gaps)
    return nc


def _consts(seq):
    ident = np.eye(128, dtype=np.float32).astype(ml_dtypes.bfloat16)
    A = np.zeros((12, 128, 128), np.float32)
    tp = np.arange(128)[:, None]
    t = np.arange(128)[None, :]
    for g, win in enumerate((2, 4, 8, 16)):
        A[g] = np.where((tp <= t) & (tp > t - win), 1.0 / win, 0.0) - (tp == t)
        A[4 + g] = np.where(tp - 128 > t - win, 1.0 / win, 0.0)
        cnt = np.minimum(t + 1, win).astype(np.float32)
        A[8 + g] = np.where((tp <= t) & (tp > t - win), 1.0 / cnt, 0.0) - (tp == t)
    masks = np.zeros((128, 2, 128), np.float32)
    masks[:, 0, :] = (tp > t)
    masks[:, 1, :] = (tp <= t)
    inv = (10000.0 ** (-np.arange(0, 64, 2, dtype=np.float32) / 64)).astype(np.float32)
    ang = np.arange(seq, dtype=np.float32)[:, None] * inv[None, :]
    return {
        "ident": ident,
        "poolA": A.astype(ml_dtypes.bfloat16),
        "masks": masks.reshape(128, 256).astype(ml_dtypes.bfloat16),
        "cos": np.cos(ang).astype(np.float32),
        "sin": np.sin(ang).astype(np.float32),
    }


def make_in_maps(inputs, cores, seq):
    f = lambda a: np.ascontiguousarray(np.asarray(a, dtype=np.float32))
    cols = np.concatenate([np.arange(64) + 64 * h for h in PERM])
    shared = {
        "pool_norm": f(inputs["pool_norm"][0]), "pool_w": f(inputs["pool_w"][0]), "pool_scale": f(inputs["pool_scale"][0]),
        "kv_norm": f(inputs["kv_norm"]), "w_kv": f(inputs["w_kv"]), "k_norm": f(inputs["k_norm"]),
        "attn_norm": f(inputs["attn_norm"][0]), "w_q": f(np.asarray(inputs["w_q"][0])[:, cols]), "q_norm": f(inputs["q_norm"][0]),
        "sinks": f(np.asarray(inputs["sinks"][0])[PERM]), "w_o": f(np.asarray(inputs["w_o"][0])[cols, :]),
        "ffn_norm": f(inputs["ffn_norm"]), "w_gu": f(inputs["w_gu"][0]), "w_down": f(inputs["w_down"][0]),
        "router_w": f(inputs["router_w"][0]), "router_b": f(inputs["router_b"][0]),
        "we_gu": f(inputs["we_gu"][0]), "we_down": f(inputs["we_down"][0]),
        "ple_gate_norm": f(inputs["ple_gate_norm"]), "ple_gate_w": f(inputs["ple_gate_w"]), "ple_w": f(inputs["ple_w"]),
    }
    shared.update(_consts(seq))
    x = np.asarray(inputs["x"]); p = np.asarray(inputs["p"])
    maps = []
    for c in cores:
        d = dict(shared)
        d["x"] = f(x[c, :seq]); d["p0"] = f(p[0, c, :seq]); d["p1"] = f(p[1, c, :seq])
        maps.append(d)
    return maps


_NC_CACHE = {}


def kernel(**inputs):
    if "full" not in _NC_CACHE:
        _NC_CACHE["full"] = build()
    nc = _NC_CACHE["full"]
    in_maps = make_in_maps(inputs, list(range(8)), SEQ)
    res = run_bass_kernel_spmd(nc, in_maps, core_ids=list(range(8)))
    return np.stack([np.asarray(r["out"], dtype=np.float32) for r in res.results], axis=0)
```
